# Optimizing a Trainium2 kernel written in Bass

```python
import math
import jax
import jax.numpy as jnp
from jax import lax
import numpy as np

D_MODEL = 2048
BATCH = 4
SEQ = 4096
DEPTH = 1

D_SSM = D_MODEL // 2
D_ATT = D_MODEL - D_SSM
SSM_HEADDIM = 64
SSM_HEADS = D_SSM // SSM_HEADDIM
SSM_GROUPS = 2
SSM_HPG = SSM_HEADS // SSM_GROUPS
SSM_STATE = 128
CONV_WIDTH = 4
CHUNK = 128
D_CONV = D_SSM + 2 * SSM_GROUPS * SSM_STATE
ATT_HEADDIM = 64
ATT_HEADS = D_ATT // ATT_HEADDIM
ATT_KV_HEADS = 2
ATT_QPK = ATT_HEADS // ATT_KV_HEADS
WINDOW = 128
D_KV = ATT_KV_HEADS * ATT_HEADDIM
PEER_HEADS = 8
PEER_KEYS = 128
PEER_EXPERTS = PEER_KEYS * PEER_KEYS
PEER_TOPK = 16
PEER_QDIM = 256
PEER_HALF = PEER_QDIM // 2
PEER_TOKEN_BLOCK = 128
N_MOD = 6
ALPHA = (2.0 * DEPTH) ** 0.25
BETA = (8.0 * DEPTH) ** -0.25
EPS = 1e-5
SPLIT_IDX = (D_SSM,
             D_SSM + D_CONV,
             D_SSM + D_CONV + SSM_HEADS,
             D_SSM + D_CONV + SSM_HEADS + D_ATT,
             D_SSM + D_CONV + SSM_HEADS + D_ATT + D_KV)
D_IN = SPLIT_IDX[-1] + D_KV

kernel_name = "hybrid_ssd_swa_peer_deepnorm_adaln"


def layer_norm(x, g, b):
    xf = x.astype(jnp.float32)
    mu = jnp.mean(xf, axis=-1, keepdims=True)
    var = jnp.mean(jnp.square(xf - mu), axis=-1, keepdims=True)
    return ((xf - mu) * lax.rsqrt(var + EPS) * g.astype(jnp.float32) + b.astype(jnp.float32)).astype(x.dtype)


def rms_norm(x, g):
    xf = x.astype(jnp.float32)
    return xf * lax.rsqrt(jnp.mean(jnp.square(xf), axis=-1, keepdims=True) + EPS) * g.astype(jnp.float32)


def causal_dwconv(x, w, b):
    out = lax.conv_general_dilated(
        x, w[:, None, :].astype(x.dtype), window_strides=(1,), padding=[(CONV_WIDTH - 1, 0)],
        dimension_numbers=('NWC', 'WIO', 'NWC'), feature_group_count=x.shape[-1])
    return out + b.astype(x.dtype)


def ssd_chunked(xh, dt, A, Bm, Cm):
    b, s = xh.shape[:2]
    nc = s // CHUNK
    xh = xh.reshape(b, nc, CHUNK, SSM_GROUPS, SSM_HPG, SSM_HEADDIM)
    dt = dt.reshape(b, nc, CHUNK, SSM_GROUPS, SSM_HPG)
    Bm = Bm.reshape(b, nc, CHUNK, SSM_GROUPS, SSM_STATE)
    Cm = Cm.reshape(b, nc, CHUNK, SSM_GROUPS, SSM_STATE)
    a_cum = jnp.cumsum(dt * A, axis=2)
    a_t = jnp.moveaxis(a_cum, 2, -1)
    seg = a_t[..., :, None] - a_t[..., None, :]
    causal = jnp.tril(jnp.ones((CHUNK, CHUNK), dtype=bool))
    decay = jnp.exp(jnp.where(causal, seg, -jnp.inf))
    cb = jnp.einsum('bclgn,bcsgn->bcgls', Cm, Bm)
    w = cb[:, :, :, None] * decay * jnp.moveaxis(dt, 2, -1)[..., None, :]
    y_diag = jnp.einsum('bcgrls,bcsgrp->bclgrp', w, xh)
    decay_to_end = jnp.exp(a_cum[:, :, -1:] - a_cum)
    states = jnp.einsum('bclgn,bclgr,bclgrp->bcgrpn', Bm, decay_to_end * dt, xh)
    chunk_decay = jnp.exp(a_cum[:, :, -1])

    def step(h, inp):
        st, dec = inp
        return dec[..., None, None] * h + st, h

    h0 = jnp.zeros((b, SSM_GROUPS, SSM_HPG, SSM_HEADDIM, SSM_STATE), jnp.float32)
    _, prev = lax.scan(step, h0, (jnp.moveaxis(states, 1, 0), jnp.moveaxis(chunk_decay, 1, 0)))
    prev = jnp.moveaxis(prev, 0, 1)
    y_off = jnp.einsum('bclgn,bcgrpn,bclgr->bclgrp', Cm, prev, jnp.exp(a_cum))
    return (y_diag + y_off).reshape(b, s, SSM_GROUPS, SSM_HPG, SSM_HEADDIM)


def sliding_window_sink_attention(q, k, v, sinks):
    f32 = jnp.float32
    b, s, _ = q.shape
    nb = s // WINDOW
    qh = q.astype(f32).reshape(b, nb, WINDOW, ATT_KV_HEADS, ATT_QPK, ATT_HEADDIM) * (ATT_HEADDIM ** -0.5)
    kh = k.astype(f32).reshape(b, nb, WINDOW, ATT_KV_HEADS, ATT_HEADDIM)
    vh = v.astype(f32).reshape(b, nb, WINDOW, ATT_KV_HEADS, ATT_HEADDIM)

    def with_prev(t):
        prev = jnp.concatenate([jnp.zeros_like(t[:, :1]), t[:, :-1]], axis=1)
        return jnp.concatenate([prev, t], axis=2)

    kb, vb = with_prev(kh), with_prev(vh)
    scores = jnp.einsum('bnqhrd,bnshd->bnhrqs', qh, kb)
    qi = jnp.arange(WINDOW)[:, None]
    kj = jnp.arange(2 * WINDOW)[None, :]
    dist = qi + WINDOW - kj
    band = (dist >= 0) & (dist < WINDOW)
    block_ids = jnp.arange(nb)[:, None, None]
    mask = band[None] & ((block_ids > 0) | (kj >= WINDOW)[None])
    scores = jnp.where(mask[None, :, None, None], scores, -jnp.inf)
    sink = sinks.astype(f32).reshape(ATT_KV_HEADS, ATT_QPK)[None, None, :, :, None, None]
    m = jnp.maximum(jnp.max(scores, axis=-1, keepdims=True), sink)
    p = jnp.exp(scores - m)
    denom = jnp.sum(p, axis=-1, keepdims=True) + jnp.exp(sink - m)
    o = jnp.einsum('bnhrqs,bnshd->bnqhrd', p / denom, vb)
    return o.reshape(b, s, D_ATT)


def hybrid_mixer(h, w_in, conv_w, conv_b, dt_bias, a_log, d_skip, ssm_norm_g, sinks, att_norm_g, w_out):
    f32 = jnp.float32
    b, s, _ = h.shape
    proj = h @ w_in
    z, xbc, dt_raw, q, k, v = jnp.split(proj, list(SPLIT_IDX), axis=-1)
    xbc = jax.nn.silu(causal_dwconv(xbc, conv_w, conv_b))
    xs, Bm, Cm = jnp.split(xbc, [D_SSM, D_SSM + SSM_GROUPS * SSM_STATE], axis=-1)
    dt = jax.nn.softplus(dt_raw.astype(f32) + dt_bias.astype(f32))
    A = -jnp.exp(a_log.astype(f32))
    xh = xs.astype(f32).reshape(b, s, SSM_GROUPS, SSM_HPG, SSM_HEADDIM)
    y = ssd_chunked(xh, dt.reshape(b, s, SSM_GROUPS, SSM_HPG), A.reshape(SSM_GROUPS, SSM_HPG),
                    Bm.astype(f32).reshape(b, s, SSM_GROUPS, SSM_STATE),
                    Cm.astype(f32).reshape(b, s, SSM_GROUPS, SSM_STATE))
    y = y + d_skip.astype(f32).reshape(SSM_GROUPS, SSM_HPG)[..., None] * xh
    y_ssd = rms_norm(y.reshape(b, s, D_SSM) * jax.nn.silu(z.astype(f32)), ssm_norm_g)
    y_att = rms_norm(sliding_window_sink_attention(q, k, v, sinks), att_norm_g)
    return jnp.concatenate([y_ssd, y_att], axis=-1).astype(h.dtype) @ w_out


def peer(h, w_q, sub_keys, u_tab, v_tab):
    f32 = jnp.float32
    b, s, d = h.shape
    blocks = h.reshape(-1, PEER_TOKEN_BLOCK, d)
    keys = sub_keys.astype(f32)

    def block(hb):
        t = hb.shape[0]
        q = (hb @ w_q).astype(f32).reshape(t, PEER_HEADS, 2, PEER_HALF)
        sc = jnp.einsum('thcd,hcnd->thcn', q, keys)
        val, idx = lax.top_k(sc, PEER_TOPK)
        cand = val[..., 0, :, None] + val[..., 1, None, :]
        cand_idx = idx[..., 0, :, None] * PEER_KEYS + idx[..., 1, None, :]
        best, pos = lax.top_k(cand.reshape(t, PEER_HEADS, PEER_TOPK * PEER_TOPK), PEER_TOPK)
        expert = jnp.take_along_axis(cand_idx.reshape(t, PEER_HEADS, PEER_TOPK * PEER_TOPK), pos, axis=-1)
        g = jax.nn.softmax(best, axis=-1)
        u = u_tab[expert]
        a = jnp.einsum('td,thkd->thk', hb, u)
        wgt = (g * jax.nn.gelu(a.astype(f32), approximate=False)).astype(hb.dtype)
        return jnp.einsum('thk,thkd->td', wgt, v_tab[expert])

    return lax.map(block, blocks).reshape(b, s, d)


def setup_inputs(seed: int = 0) -> dict:
    key = jax.random.key(seed)
    ks = jax.random.split(key, 24)
    f32 = jnp.float32
    L = DEPTH

    def nrm(k, shape, scale):
        return jax.random.normal(k, shape, f32) * scale

    x = nrm(ks[0], (BATCH, SEQ, D_MODEL), 1.0)
    c = nrm(ks[1], (BATCH, D_MODEL), 1.0)
    w_ada = nrm(ks[2], (L, D_MODEL, N_MOD * D_MODEL), 0.5 * D_MODEL ** -0.5)
    b_ada = nrm(ks[3], (L, N_MOD * D_MODEL), 0.01)
    w_in = nrm(ks[4], (L, D_MODEL, D_IN), D_MODEL ** -0.5)
    w_in = w_in.at[:, :, SPLIT_IDX[-1]:].multiply(BETA)
    conv_w = nrm(ks[5], (L, CONV_WIDTH, D_CONV), CONV_WIDTH ** -0.5)
    conv_b = nrm(ks[6], (L, D_CONV), 0.02)
    dt0 = jnp.exp(jax.random.uniform(ks[7], (L, SSM_HEADS), f32, math.log(1e-3), math.log(1e-1)))
    dt_bias = dt0 + jnp.log(-jnp.expm1(-dt0))
    a_log = jnp.log(jax.random.uniform(ks[8], (L, SSM_HEADS), f32, 1.0, 16.0))
    d_skip = 1.0 + nrm(ks[9], (L, SSM_HEADS), 0.02)
    ssm_norm_g = 1.0 + nrm(ks[10], (L, D_SSM), 0.02)
    attn_sinks = nrm(ks[11], (L, ATT_HEADS), 0.5)
    attn_norm_g = 1.0 + nrm(ks[12], (L, D_ATT), 0.02)
    w_out = nrm(ks[13], (L, D_SSM + D_ATT, D_MODEL), BETA * (D_SSM + D_ATT) ** -0.5)
    ln1_g = 1.0 + nrm(ks[14], (L, D_MODEL), 0.02)
    ln1_b = nrm(ks[15], (L, D_MODEL), 0.02)
    peer_w_q = nrm(ks[16], (L, D_MODEL, PEER_HEADS * PEER_QDIM), D_MODEL ** -0.5)
    peer_sub_keys = nrm(ks[17], (L, PEER_HEADS, 2, PEER_KEYS, PEER_HALF), PEER_HALF ** -0.5)
    peer_u = nrm(ks[18], (L, PEER_EXPERTS, D_MODEL), D_MODEL ** -0.5)
    peer_v = nrm(ks[19], (L, PEER_EXPERTS, D_MODEL), BETA)
    ln2_g = 1.0 + nrm(ks[20], (L, D_MODEL), 0.02)
    ln2_b = nrm(ks[21], (L, D_MODEL), 0.02)
    return {'x': x, 'c': c, 'w_ada': w_ada, 'b_ada': b_ada, 'w_in': w_in, 'conv_w': conv_w,
            'conv_b': conv_b, 'dt_bias': dt_bias, 'a_log': a_log, 'd_skip': d_skip,
            'ssm_norm_g': ssm_norm_g, 'attn_sinks': attn_sinks, 'attn_norm_g': attn_norm_g,
            'w_out': w_out, 'ln1_g': ln1_g, 'ln1_b': ln1_b, 'peer_w_q': peer_w_q,
            'peer_sub_keys': peer_sub_keys, 'peer_u': peer_u, 'peer_v': peer_v,
            'ln2_g': ln2_g, 'ln2_b': ln2_b}


def reference(x, c, w_ada, b_ada, w_in, conv_w, conv_b, dt_bias, a_log, d_skip, ssm_norm_g,
              attn_sinks, attn_norm_g, w_out, ln1_g, ln1_b, peer_w_q, peer_sub_keys, peer_u,
              peer_v, ln2_g, ln2_b):
    for l in range(DEPTH):
        mod = jax.nn.silu(c) @ w_ada[l] + b_ada[l]
        shift1, scale1, gate1, shift2, scale2, gate2 = [m[:, None, :] for m in jnp.split(mod, N_MOD, axis=-1)]
        h = x * (1.0 + scale1) + shift1
        y = hybrid_mixer(h, w_in[l], conv_w[l], conv_b[l], dt_bias[l], a_log[l], d_skip[l],
                         ssm_norm_g[l], attn_sinks[l], attn_norm_g[l], w_out[l])
        x = layer_norm(ALPHA * x + gate1 * y, ln1_g[l], ln1_b[l])
        h = x * (1.0 + scale2) + shift2
        y = peer(h, peer_w_q[l], peer_sub_keys[l], peer_u[l], peer_v[l])
        x = layer_norm(ALPHA * x + gate2 * y, ln2_g[l], ln2_b[l])
    return x
```

```python
import contextlib
import numpy as np
import concourse.bass as bass
import concourse.mybir as mybir

F32 = mybir.dt.float32
BF16 = mybir.dt.bfloat16
AF = mybir.ActivationFunctionType
ALU = mybir.AluOpType
AX = mybir.AxisListType

ENGS = ("tensor", "vector", "scalar", "gpsimd", "sync")


class Prog:
    def __init__(self, nc):
        self.nc = nc
        self.ops = []
        self.last_w = {}
        self.readers = {}
        self.eng_last = {e: None for e in ENGS}
        self.dma_keys = {}

    def op(self, eng, fn, reads=(), writes=(), dma=None, multi_w=False):
        i = len(self.ops)
        deps = set()
        reads = [r if isinstance(r, (str, tuple)) else r.name for r in reads]
        writes = [w if isinstance(w, (str, tuple)) else w.name for w in writes]
        for r in reads:
            deps.update(self.last_w.get(r, ()))
        for w in writes:
            deps.update(self.last_w.get(w, ()))
            deps.update(self.readers.get(w, ()))
        for r in reads:
            self.readers.setdefault(r, []).append(i)
        for w in writes:
            if multi_w:
                self.last_w.setdefault(w, []).append(i)
            else:
                self.last_w[w] = [i]
            self.readers[w] = []
        deps.discard(i)
        self.ops.append(dict(eng=eng, fn=fn, deps=deps, dma=dma))
        if dma is not None:
            self.dma_keys.setdefault(dma, []).append(i)
        return i

    def barrier(self):
        allprev = set(range(len(self.ops)))
        need = set()
        for e in ENGS:
            for j in range(len(self.ops) - 1, -1, -1):
                if self.ops[j]["eng"] == e and self.ops[j]["dma"] is None:
                    need.add(j)
                    break
        for k, lst in self.dma_keys.items():
            need.add(lst[-1])
        for e in ENGS:
            i = len(self.ops)
            self.ops.append(dict(eng=e, fn=None, deps=set(need), dma=None))
        self.last_w = {}
        self.readers = {}

    def emit(self, final_wait=()):
        nc = self.nc
        ops = self.ops
        sig = [False] * len(ops)
        for i, o in enumerate(ops):
            for d in o["deps"]:
                od = ops[d]
                if od["dma"] is not None:
                    continue
                if od["eng"] == "tensor" and o["eng"] == "tensor" and o["dma"] is None:
                    continue
                sig[d] = True
        cnt = {e: 0 for e in ENGS}
        sigval = {}
        for i, o in enumerate(ops):
            if o["dma"] is not None:
                continue
            if sig[i]:
                cnt[o["eng"]] += 1
                sigval[i] = cnt[o["eng"]]
        dmaval = {}
        for k, lst in self.dma_keys.items():
            for n, i in enumerate(lst):
                dmaval[i] = 16 * (n + 1)
        with contextlib.ExitStack() as st:
            esem = {e: st.enter_context(nc.semaphore("s_" + e)) for e in ENGS}
            dsem = {k: st.enter_context(nc.semaphore("d_%d" % n)) for n, k in enumerate(self.dma_keys)}
            block = st.enter_context(nc.Block())
            per = {e: [i for i, o in enumerate(ops) if o["eng"] == e] for e in ENGS}

            def run(e, eng):
                known = {}
                for i in per[e]:
                    o = ops[i]
                    waits = {}
                    for d in o["deps"]:
                        od = ops[d]
                        if od["dma"] is not None:
                            s, v = dsem[od["dma"]], dmaval[d]
                        else:
                            if od["fn"] is None:
                                continue
                            if od["eng"] == "tensor" and e == "tensor" and o["dma"] is None:
                                continue
                            s, v = esem[od["eng"]], sigval[d]
                        key = id(s)
                        if waits.get(key, (None, 0))[1] < v:
                            waits[key] = (s, v)
                    for key, (s, v) in waits.items():
                        if known.get(key, 0) < v:
                            eng.wait_ge(s, v)
                            known[key] = v
                    if o["fn"] is None:
                        continue
                    ins = o["fn"](eng)
                    if o["dma"] is not None:
                        ins.then_inc(dsem[o["dma"]], 16)
                    elif sig[i]:
                        ins.then_inc(esem[e], 1)
                if e == "sync":
                    for k in final_wait:
                        lst = self.dma_keys[k]
                        eng.wait_ge(dsem[k], 16 * len(lst))

            @block.tensor
            def _(eng):
                run("tensor", eng)

            @block.vector
            def _(eng):
                run("vector", eng)

            @block.scalar
            def _(eng):
                run("scalar", eng)

            @block.gpsimd
            def _(eng):
                run("gpsimd", eng)

            @block.sync
            def _(eng):
                run("sync", eng)


from concourse.bass_utils import run_bass_kernel_spmd

D = 2048
KC = 16
T = 128
ALPHA = 2.0 ** 0.25
EPS = 1e-5
NEB = 64
Z0, XS0, B0_, C0_, DT0, Q0, K0, V0 = 0, 1024, 2048, 2304, 2560, 2576, 3600, 3728
BZ, BXS, BB, BC, BQ, BKA, BKB, BV, BDT = 0, 8, 16, 18, 20, 28, 29, 30, 31


def build(nt=16, npre=16, stop=None):
    nc = bass.Bass("TRN2", target_bir_lowering=False)
    P = Prog(nc)
    ntok = nt * T
    din = lambda n, s, d=F32: nc.dram_tensor(n, list(s), d, kind="ExternalInput").ap()
    xm = din("xm", [ntok, D]); xp = din("xp", [max(npre, 1) * T, D])
    cT = din("cT", [128, 16]); flag = din("flag", [128, 1])
    idn_d = din("idn", [128, 128]); tri_d = din("tri", [128, 128]); mst_d = din("mst", [128, 128])
    w_ada = din("w_ada", [D, 6 * D]); b_ada = din("b_ada", [6 * D])
    wp = din("wp", [D, 4096]); convw = din("convw", [128, 12, 4]); convb = din("convb", [128, 12])
    dtb = din("dt_bias", [16]); alog = din("a_log", [16]); dsk = din("d_skip", [16]); snk = din("sinks", [16])
    gcat = din("gcat", [128, 16]); w_out = din("w_out", [D, D])
    l1g = din("ln1_g", [D]); l1b = din("ln1_b", [D]); l1gT = din("l1gT", [128, 16]); l1bT = din("l1bT", [128, 16])
    w_q = din("w_q", [D, D]); keys = din("keys", [16, 128, 128])
    pu = din("peer_u", [16384, D]); pv = din("peer_v", [16384, D])
    l2g = din("ln2_g", [D]); l2b = din("ln2_b", [D])
    out = nc.dram_tensor("out", [ntok, D], F32, kind="ExternalOutput").ap()
    dint = lambda n, s: nc.dram_tensor(n, list(s), BF16, kind="Internal").ap()
    WIN = dint("WIN", [32, 128, 16, 128]); WOUT = dint("WOUT", [8, 128, 16, 256]); WQ = dint("WQ", [16, 128, 16, 128])
    UT = dint("UT", [NEB, 128, 16, 256]); VV = dint("VV", [NEB, 128, 2, D])

    ctr = [0]

    def nm(p):
        ctr[0] += 1
        return "%s_%d" % (p, ctr[0])

    def op(eng, name, kw, R=(), W=(), **k2):
        return P.op(eng, lambda e: getattr(e, name)(**kw), reads=R, writes=W, **k2)

    def dma(outap, inap, R=(), W=(), key=None, q="sync", **k2):
        return P.op(q, lambda e: e.dma_start(out=outap, in_=inap), reads=R, writes=W, dma=key, **k2)

    def bc(ap, shape):
        return ap.broadcast_to(list(shape))

    with contextlib.ExitStack() as top:
        TT_ = lambda n, s, d=F32: top.enter_context(nc.sbuf_tensor("sb_" + n, list(s), d))
        acc = top.enter_context(nc.psum_tensor("acc", [128, 2048], F32))
        rg = top.enter_context(nc.psum_tensor("rg", [128, 2048], F32))
        rgb = rg[:].bitcast(BF16)
        rstate = dict(n=0)

        def rq():
            n = rstate["n"]; rstate["n"] = n + 1
            b = n % 4; qq = (n // 4) % 4
            return b * 4 + qq, [("rg", b)]

        def rb():
            n = rstate["n"]; rstate["n"] = n + 1
            b = n % 4
            return b, [("rg", b)]

        ACCK = [("acc", j) for j in range(4)]
        idn = TT_("idn", [128, 128]); tri = TT_("tri", [128, 128]); mst = TT_("mst", [128, 128])
        idb = TT_("idb", [128, 128], BF16); mprevF = TT_("mprevF", [128, 128]); ones = TT_("ones", [128, 128])
        onesb = TT_("onesb", [128, 1], BF16); flg = TT_("flg", [128, 1])
        modT = TT_("modT", [128, 64])
        A2 = TT_("A2", [128, 16]); B2 = TT_("B2", [128, 16])
        KT = TT_("KT", [128, 16, 128])
        cw = TT_("cw", [128, 12, 4]); cb = TT_("cb", [128, 12])
        dtb_bc = TT_("dtb_bc", [128, 16]); A_bc = TT_("A_bc", [128, 16]); dsk_bc = TT_("dsk_bc", [128, 16])
        esink = TT_("esink", [128, 16])
        dma(idn[:], idn_d, W=[idn], key="c0"); dma(tri[:], tri_d, W=[tri], key="c1"); dma(mst[:], mst_d, W=[mst], key="c2")
        dma(flg[:], flag, W=[flg], key="c3"); dma(cw[:], convw, W=[cw], key="c4"); dma(cb[:], convb, W=[cb], key="c5")
        dma(dtb_bc[:], dtb.partition_broadcast(128), W=[dtb_bc], key="c6")
        dma(A_bc[:], alog.partition_broadcast(128), W=[A_bc], key="c7")
        dma(dsk_bc[:], dsk.partition_broadcast(128), W=[dsk_bc], key="c8")
        dma(esink[:], snk.partition_broadcast(128), W=[esink], key="c9")
        op("vector", "tensor_copy", dict(out=idb[:], in_=idn[:]), R=[idn], W=[idb])
        op("vector", "memset", dict(ap=ones[:], constant=1.0), W=[ones])
        op("vector", "memset", dict(ap=onesb[:], constant=1.0), W=[onesb])
        op("vector", "tensor_scalar", dict(out=mprevF[:], in0=mst[:], scalar1=flg[:, 0:1], scalar2=None, op0=ALU.mult), R=[mst, flg], W=[mprevF])
        op("scalar", "activation", dict(out=A_bc[:], in_=A_bc[:], func=AF.Exp), R=[A_bc], W=[A_bc])
        op("vector", "tensor_scalar", dict(out=A_bc[:], in0=A_bc[:], scalar1=-1.0, scalar2=None, op0=ALU.mult), R=[A_bc], W=[A_bc])
        op("scalar", "activation", dict(out=esink[:], in_=esink[:], func=AF.Exp), R=[esink], W=[esink])

        with contextlib.ExitStack() as pro:
            PT = lambda n, s, d=F32: pro.enter_context(nc.sbuf_tensor("sp_" + n, list(s), d))
            cTt = PT("cTt", [128, 16]); SCb = PT("SCb", [128, 16, 128])
            g1bc = PT("g1bc", [128, D]); g2bc = PT("g2bc", [128, D]); gct = PT("gct", [128, 16])
            wab = [PT("wab%d" % i, [128, 4, 512]) for i in range(3)]
            bab = [PT("bab%d" % i, [128, 512]) for i in range(2)]
            mrow = [PT("mrow%d" % i, [128, 512]) for i in range(2)]
            st32 = [PT("st32_%d" % i, [128, 4096]) for i in range(2)]
            st16 = [PT("st16_%d" % i, [128, 4096], BF16) for i in range(2)]
            ut16 = [PT("ut16_%d" % i, [128, 4096], BF16) for i in range(2)]
            l1gt = PT("l1gt", [128, 16]); l1bt = PT("l1bt", [128, 16])
            dma(cTt[:], cT, W=[cTt], key="p0"); dma(gct[:], gcat, W=[gct], key="p1")
            dma(l1gt[:], l1gT, W=[l1gt], key="p2"); dma(l1bt[:], l1bT, W=[l1bt], key="p3")
            op("scalar", "activation", dict(out=cTt[:], in_=cTt[:], func=AF.Silu), R=[cTt], W=[cTt])
            op("vector", "tensor_copy", dict(out=SCb[:], in_=bc(cTt[:].unsqueeze(2), [128, 16, 128])), R=[cTt], W=[SCb])

            def finish_early():
                dma(out[0:128, 0:64], modT[:], R=[modT], key="o0", q="scalar")
                dma(out[0:128, 64:80], A2[:], R=[A2], key="o0", q="scalar")
                dma(out[0:128, 80:96], B2[:], R=[B2], key="o0", q="scalar")
                dma(out[0:128, 128:256], KT[:, 3, :], R=[KT], key="o0", q="scalar")
                P.barrier()
                P.emit(final_wait=["o0"])
                return nc
            if stop == "pA":
                return finish_early()
            for hc in range(16):
                kb_ = st32[hc % 2]
                dma(kb_[:, 0:128], keys[hc], W=[kb_], key="pk%d" % (hc % 2))
                q, qk = rq()
                op("tensor", "transpose", dict(out=rg[:, q * 128:(q + 1) * 128], in_=kb_[:, 0:128], identity=idn[:]), R=[kb_, idn], W=qk)
                op("vector", "tensor_copy", dict(out=KT[:, hc, :], in_=rg[:, q * 128:(q + 1) * 128]), R=qk, W=[KT])
            if stop == "p0":
                return finish_early()
            for g in range(24):
                b, bk = rb()
                bb = bab[g % 2]
                dma(bb[:], b_ada[g * 512:(g + 1) * 512].partition_broadcast(128), W=[bb], key="pb%d" % (g % 2))
                for kq in range(4):
                    wb = wab[(g * 4 + kq) % 3]
                    dma(wb[:], w_ada[kq * 512:(kq + 1) * 512, g * 512:(g + 1) * 512].rearrange("(k p) c -> p k c", p=128),
                        W=[wb], key="pw%d" % ((g * 4 + kq) % 3))
                    for k in range(4):
                        kc = kq * 4 + k
                        op("tensor", "matmul", dict(out=rg[:, b * 512:(b + 1) * 512], lhsT=SCb[:, kc, :], rhs=wb[:, k, :],
                                                    start=(kc == 0), stop=(kc == 15)), R=[SCb, wb], W=bk)
                gi = g // 4
                if gi == 2:
                    dst, dk = g1bc[:, (g % 4) * 512:(g % 4 + 1) * 512], g1bc
                elif gi == 5:
                    dst, dk = g2bc[:, (g % 4) * 512:(g % 4 + 1) * 512], g2bc
                else:
                    mr = mrow[g % 2]
                    dst, dk = mr[:], mr
                op("vector", "tensor_tensor", dict(out=dst, in0=rg[:, b * 512:(b + 1) * 512], in1=bb[:], op=ALU.add), R=bk + [bb], W=[dk])
                if gi in (0, 1, 3, 4):
                    col0 = {0: 0, 1: 16, 3: 32, 4: 48}[gi] + (g % 4) * 4
                    b2, bk2 = rb()
                    for j in range(4):
                        op("tensor", "transpose", dict(out=rg[:, b2 * 512 + j * 128: b2 * 512 + (j + 1) * 128], in_=mr[:, j * 128:(j + 1) * 128], identity=idn[:]),
                           R=[mr, idn], W=bk2)
                    src = rg[:, b2 * 512:(b2 + 1) * 512].rearrange("p (j t) -> p j t", t=128)[:, :, 0]
                    if gi in (1, 4):
                        op("vector", "tensor_scalar", dict(out=modT[:, col0:col0 + 4], in0=src, scalar1=1.0, scalar2=None, op0=ALU.add), R=bk2, W=[modT])
                    else:
                        op("vector", "tensor_copy", dict(out=modT[:, col0:col0 + 4], in_=src), R=bk2, W=[modT])
            op("vector", "tensor_tensor", dict(out=A2[:], in0=l1gt[:], in1=modT[:, 48:64], op=ALU.mult), R=[l1gt, modT], W=[A2])
            op("vector", "tensor_tensor", dict(out=B2[:], in0=l1bt[:], in1=modT[:, 48:64], op=ALU.mult), R=[l1bt, modT], W=[B2])
            op("vector", "tensor_tensor", dict(out=B2[:], in0=B2[:], in1=modT[:, 32:48], op=ALU.add), R=[B2, modT], W=[B2])
            if stop == "p1":
                return finish_early()
            pc = [0]

            def stage(src_ap, shape3, mul_bc=None, mul_pp=None, dst=None, dkey=None):
                i = pc[0] % 2; pc[0] += 1
                s32, s16 = st32[i], st16[i]
                n = shape3[1] * shape3[2]
                v32 = s32[:, 0:n].rearrange("p (a b) -> p a b", b=shape3[2])
                v16 = s16[:, 0:n].rearrange("p (a b) -> p a b", b=shape3[2])
                dma(v32, src_ap, W=[s32], key="pl%d" % i)
                if mul_bc is not None:
                    op("vector", "tensor_tensor", dict(out=v32, in0=v32, in1=mul_bc, op=ALU.mult), R=[s32, g1bc, g2bc], W=[s32])
                if mul_pp is not None:
                    op("gpsimd", "tensor_tensor", dict(out=v16, in0=v32, in1=mul_pp, op=ALU.mult), R=[s32, gct], W=[s16])
                else:
                    op("scalar", "copy", dict(out=v16, in_=v32), R=[s32], W=[s16])
                if dst is not None:
                    dma(dst, v16, R=[s16], W=[dkey], key="ps%d" % i, q="scalar", multi_w=True)
                return s16, v16

            for b in range(32):
                stage(wp[:, b * 128:(b + 1) * 128].rearrange("(k p) c -> p k c", p=128), [128, 16, 128], dst=WIN[b], dkey="WIN")
            if stop == "p2":
                return finish_early()
            for b in range(16):
                stage(w_q[:, b * 128:(b + 1) * 128].rearrange("(k p) c -> p k c", p=128), [128, 16, 128], dst=WQ[b], dkey="WQ")
            for b in range(8):
                stage(w_out[:, b * 256:(b + 1) * 256].rearrange("(k p) c -> p k c", p=128), [128, 16, 256],
                      mul_bc=bc(g1bc[:, b * 256:(b + 1) * 256].unsqueeze(1), [128, 16, 256]),
                      mul_pp=bc(gct[:].unsqueeze(2), [128, 16, 256]), dst=WOUT[b], dkey="WOUT")
            if stop == "p3":
                return finish_early()
            for b in range(NEB):
                stage(pv[b * 256:(b + 1) * 256, :].rearrange("(c p) d -> p c d", p=128), [128, 2, D],
                      mul_bc=bc(g2bc[:].unsqueeze(1), [128, 2, D]), dst=VV[b], dkey="VV")
            if stop == "p4":
                return finish_early()
            for b in range(NEB):
                s16, v16 = stage(pu[b * 256:(b + 1) * 256, :].rearrange("(c p) d -> p c d", p=128), [128, 2, D])
                u16 = ut16[b % 2]
                uv = u16[:].rearrange("p (k e) -> p k e", e=256)
                for kg in range(4):
                    bnk, bk = rb()
                    for k in range(4):
                        kc = kg * 4 + k
                        for ci in range(2):
                            o0 = bnk * 1024 + k * 256 + ci * 128
                            op("tensor", "transpose", dict(out=rgb[:, o0:o0 + 128], in_=v16[:, ci, kc * 128:(kc + 1) * 128], identity=idb[:]),
                               R=[s16, idb], W=bk)
                    op("vector", "tensor_copy", dict(out=u16[:, kg * 1024:(kg + 1) * 1024], in_=rgb[:, bnk * 1024:(bnk + 1) * 1024]), R=bk, W=[u16])
                dma(UT[b], uv, R=[u16], W=["UT"], key="pu%d" % (b % 2), q="scalar", multi_w=True)
            P.barrier()
            if stop == "pro":
                dma(out[0:128, 0:64], modT[:], R=[modT], key="o0", q="scalar")
                dma(out[0:128, 64:80], A2[:], R=[A2], key="o0", q="scalar")
                dma(out[0:128, 80:96], B2[:], R=[B2], key="o0", q="scalar")
                P.emit(final_wait=["o0"])
                return nc
        MT = TT_
        xt = [MT("xt%d" % i, [128, D]) for i in range(1)]
        win_r = [MT("winr%d" % i, [128, 16, 128], BF16) for i in range(3)]
        wout_r = [MT("woutr%d" % i, [128, 16, 256], BF16) for i in range(2)]
        wq_r = [MT("wqr%d" % i, [128, 16, 128], BF16) for i in range(3)]
        ut_r = [MT("utr%d" % i, [128, 16, 256], BF16) for i in range(2)]
        vv_r = [MT("vvr%d" % i, [128, 2, D], BF16) for i in range(2)]
        hT = MT("hT", [128, 16, 128], BF16); h2T = hT
        XC = MT("XC", [128, 12, 131]); S = MT("S", [128, 1024])
        KTb = MT("KTb", [128, 2, 2, 128], BF16)
        VB = MT("VB", [128, 2, 128], BF16)
        Bs = [MT("BIG%d" % i, [128, D]) for i in range(8)]
        sm = MT("sm", [128, 256])
        Vt = MT("Vt", [128, 16, 16]); BVt = MT("BVt", [128, 8, 16]); wk = MT("wk", [128, 256])
        mx8 = MT("mx8", [128, 16, 8])
        G = [MT("G0", [128, 1024])] * 2
        Pm = MT("Pm", [128, 1024]); Mm = MT("Mm", [128, 1024]); M2 = MT("M2", [128, 2048], BF16)
        gel = [MT("gel%d" % i, [128, 512]) for i in range(2)]
        wg = [MT("wg%d" % i, [128, 512], BF16) for i in range(2)]
        wgT = [MT("wgT%d" % i, [128, 4, 128], BF16) for i in range(2)]
        G0b = G[0][:].bitcast(BF16); G1b = G0b[:, 1024:2048]
        qT = G0b[:, 0:1024].rearrange("p (a b) -> p a b", b=128); qTk = G[0]
        pTm = [(G1b[:, 0:512], G[1]), (G1b[:, 512:1024], G[1])]
        pTe = [(Pm[:, 512:1024], Pm), (Mm[:, 512:1024], Mm)]
        op("vector", "memset", dict(ap=XC[:], constant=0.0), W=[XC])
        op("vector", "memset", dict(ap=S[:], constant=0.0), W=[S])
        op("vector", "memset", dict(ap=KTb[:], constant=0.0), W=[KTb])
        op("vector", "memset", dict(ap=VB[:], constant=0.0), W=[VB])

        sc_ = dict(win=0, wout=0, wq=0, ut=0, vv=0, x=0)

        def ld_win(b):
            i = sc_["win"] % 3; sc_["win"] += 1
            dma(win_r[i][:], WIN[b], R=["WIN"], W=[win_r[i]], key="win%d" % i)
            return win_r[i]

        def ld_wout(b):
            i = sc_["wout"] % 2; sc_["wout"] += 1
            dma(wout_r[i][:], WOUT[b], R=["WOUT"], W=[wout_r[i]], key="wout%d" % i)
            return wout_r[i]

        def ld_wq(b):
            i = sc_["wq"] % 3; sc_["wq"] += 1
            dma(wq_r[i][:], WQ[b], R=["WQ"], W=[wq_r[i]], key="wq%d" % i)
            return wq_r[i]

        def _bf(t):
            return t[:].bitcast(BF16)
        ut_slots = [(ut_r[0][:], ut_r[0]), (ut_r[1][:], ut_r[1])] + \
                   [(_bf(Bs[i]).rearrange("p (k e) -> p k e", e=256), Bs[i]) for i in (1, 3, 4)]
        vv_slots = [(vv_r[0][:], vv_r[0]), (vv_r[1][:], vv_r[1])] + \
                   [(_bf(Bs[i]).rearrange("p (c d) -> p c d", d=D), Bs[i]) for i in (5, 6, 0)]

        def ld_ut(b):
            i = sc_["ut"] % 5; sc_["ut"] += 1
            view, kt = ut_slots[i]
            dma(view, UT[b], R=["UT"], W=[kt], key="ut%d" % i)
            return view, kt

        def ld_vv(b):
            i = sc_["vv"] % 5; sc_["vv"] += 1
            view, kt = vv_slots[i]
            dma(view, VV[b], R=["VV"], W=[kt], key="vv%d" % i)
            return view, kt

        def rstd_from(var_ap, out_ap, keyt):
            op("vector", "tensor_scalar", dict(out=out_ap, in0=var_ap, scalar1=EPS, scalar2=None, op0=ALU.add), R=[keyt], W=[keyt])
            op("scalar", "activation", dict(out=out_ap, in_=out_ap, func=AF.Ln), R=[keyt], W=[keyt])
            op("scalar", "activation", dict(out=out_ap, in_=out_ap, func=AF.Exp, scale=-0.5), R=[keyt], W=[keyt])

        def transposes_f32(src_fn, n, dst_fn, dkeys, rkeys, evac="vector"):
            j = 0
            while j < n:
                cnt = min(4, n - j)
                b, bk = rb()
                for k in range(cnt):
                    op("tensor", "transpose", dict(out=rg[:, b * 512 + k * 128: b * 512 + (k + 1) * 128], in_=src_fn(j + k), identity=idn[:]),
                       R=rkeys + [idn], W=bk)
                if evac == "vector":
                    op("vector", "tensor_copy", dict(out=dst_fn(j, cnt), in_=rg[:, b * 512: b * 512 + cnt * 128]), R=bk, W=dkeys)
                else:
                    op("scalar", "copy", dict(out=dst_fn(j, cnt), in_=rg[:, b * 512: b * 512 + cnt * 128]), R=bk, W=dkeys)
                j += cnt

        def mixer_tile(tau):
            full = tau >= 0
            par = tau % 2
            xsrc = xm[tau * T:(tau + 1) * T, :] if full else xp[(npre + tau) * T:(npre + tau + 1) * T, :]
            xi = 0
            xtl = xt[xi]
            dma(xtl[:], xsrc, W=[xtl], key="x%d" % xi)
            if tau == 0:
                op("vector", "tensor_scalar", dict(out=S[:], in0=S[:], scalar1=flg[:, 0:1], scalar2=None, op0=ALU.mult), R=[S, flg], W=[S])
                op("vector", "tensor_scalar", dict(out=XC[:], in0=XC[:], scalar1=flg[:, 0:1], scalar2=None, op0=ALU.mult), R=[XC, flg], W=[XC])
            for j0 in range(0, 16, 4):
                b, bk = rb()
                for k in range(4):
                    kc = j0 + k
                    op("tensor", "transpose", dict(out=rg[:, b * 512 + k * 128:b * 512 + (k + 1) * 128], in_=xtl[:, kc * 128:(kc + 1) * 128], identity=idn[:]),
                       R=[xtl, idn], W=bk)
                for k in range(4):
                    kc = j0 + k
                    op("scalar", "activation", dict(out=hT[:, kc, :], in_=rg[:, b * 512 + k * 128:b * 512 + (k + 1) * 128], func=AF.Identity,
                                                    scale=modT[:, 16 + kc:17 + kc], bias=modT[:, kc:kc + 1]), R=bk + [modT], W=[hT])
            R_, Dm, cacc, ctmp, XA, B5, B6, vv_ = Bs
            XS = B5[:, 0:1024]; XSd = B5[:, 1024:2048]
            szT = B6[:, 0:1024].rearrange("p (a b) -> p a b", b=128); sz = B6[:, 1024:2048]
            XAv = XA[:, 0:1536].rearrange("p (a b) -> p a b", b=128)

            def proj(blk):
                w = ld_win(blk)
                q, qk = rq()
                for kc in range(16):
                    op("tensor", "matmul", dict(out=rg[:, q * 128:(q + 1) * 128], lhsT=w[:, kc, :], rhs=hT[:, kc, :], start=(kc == 0), stop=(kc == 15)),
                       R=[w, hT], W=qk)
                return rg[:, q * 128:(q + 1) * 128], qk

            nconv = 12 if (full or tau == -1) else 10
            for j in range(nconv):
                ps, qk = proj(BXS + j)
                op("vector", "tensor_copy", dict(out=XC[:, j, 3:131], in_=ps), R=qk, W=[XC])
            ca = cacc[:, 0:1536].rearrange("p (a b) -> p a b", b=128)[:, 0:nconv, :]
            ct = ctmp[:, 0:1536].rearrange("p (a b) -> p a b", b=128)[:, 0:nconv, :]
            for k in range(4):
                dst, dk = (ca, cacc) if k == 0 else (ct, ctmp)
                op("vector", "tensor_tensor", dict(out=dst, in0=XC[:, 0:nconv, k:k + 128], in1=bc(cw[:, 0:nconv, k:k + 1], [128, nconv, 128]), op=ALU.mult),
                   R=[XC, cw], W=[dk])
                if k > 0:
                    op("vector", "tensor_tensor", dict(out=ca, in0=ca, in1=ct, op=ALU.add), R=[cacc, ctmp], W=[cacc])
            op("vector", "tensor_tensor", dict(out=ca, in0=ca, in1=bc(cb[:, 0:nconv].unsqueeze(2), [128, nconv, 128]), op=ALU.add), R=[cacc, cb], W=[cacc])
            op("scalar", "activation", dict(out=XAv[:, 0:nconv, :], in_=ca, func=AF.Silu), R=[cacc], W=[XA])
            op("vector", "tensor_copy", dict(out=XC[:, :, 0:3], in_=XC[:, :, 128:131]), R=[XC], W=[XC])
            transposes_f32(lambda j: XAv[:, j, :], 8, lambda j, c: B5[:, j * 128:(j + c) * 128], [B5], [XA])
            Btok = sm[:, 0:256]
            Btok = Mm[:, 0:256]
            transposes_f32(lambda j: XAv[:, 8 + j, :], 2, lambda j, c: Mm[:, j * 128:(j + c) * 128], [Mm], [XA])
            ps, qk = proj(BDT)
            dtT = Pm[:, 0:128]
            op("vector", "tensor_copy", dict(out=dtT, in_=ps), R=qk, W=[Pm])
            q, qk = rq()
            op("tensor", "transpose", dict(out=rg[:, q * 128:(q + 1) * 128], in_=dtT, identity=idn[:]), R=[Pm, idn], W=qk)
            dt = sm[:, 0:16]; dtA = sm[:, 16:32]; acum = sm[:, 32:48]; alast = sm[:, 48:64]; dte = sm[:, 64:80]; ea = sm[:, 80:96]; cd = sm[:, 96:112]
            op("vector", "tensor_tensor", dict(out=dt, in0=rg[:, q * 128:q * 128 + 16], in1=dtb_bc[:], op=ALU.add), R=qk + [dtb_bc], W=[sm])
            op("scalar", "activation", dict(out=dt, in_=dt, func=AF.Exp), R=[sm], W=[sm])
            op("vector", "tensor_scalar", dict(out=dt, in0=dt, scalar1=1.0, scalar2=None, op0=ALU.add), R=[sm], W=[sm])
            op("scalar", "activation", dict(out=dt, in_=dt, func=AF.Ln), R=[sm], W=[sm])
            op("vector", "tensor_tensor", dict(out=dtA, in0=dt, in1=A_bc[:], op=ALU.mult), R=[sm, A_bc], W=[sm])
            q, qk = rq()
            op("tensor", "matmul", dict(out=rg[:, q * 128:q * 128 + 16], lhsT=tri[:], rhs=dtA, start=True, stop=True), R=[tri, sm], W=qk)
            op("tensor", "matmul", dict(out=rg[:, q * 128 + 16:q * 128 + 32], lhsT=ones[:], rhs=dtA, start=True, stop=True), R=[ones, sm], W=qk)
            op("vector", "tensor_copy", dict(out=sm[:, 32:64], in_=rg[:, q * 128:q * 128 + 32]), R=qk, W=[sm])
            op("vector", "tensor_tensor", dict(out=dte, in0=alast, in1=acum, op=ALU.subtract), R=[sm], W=[sm])
            op("scalar", "activation", dict(out=dte, in_=dte, func=AF.Exp), R=[sm], W=[sm])
            op("vector", "tensor_tensor", dict(out=dte, in0=dte, in1=dt, op=ALU.mult), R=[sm], W=[sm])
            op("scalar", "activation", dict(out=ea, in_=acum, func=AF.Exp), R=[sm], W=[sm])
            op("scalar", "activation", dict(out=cd, in_=alast, func=AF.Exp), R=[sm], W=[sm])
            if full:
                for j in range(8):
                    ps, qk = proj(BZ + j)
                    op("scalar", "activation", dict(out=szT[:, j, :], in_=ps, func=AF.Silu), R=qk, W=[B6])
                for j in range(8):
                    ps, qk = proj(BQ + j)
                    op("scalar", "activation", dict(out=qT[:, j, :], in_=ps, func=AF.Identity, scale=0.125), R=qk, W=[qTk])
            if full or tau == -1:
                for ab, blk in ((0, BKA), (1, BKB)):
                    ps, qk = proj(blk)
                    op("vector", "tensor_copy", dict(out=KTb[:, ab, par, :], in_=ps), R=qk, W=[KTb])
                ps, qk = proj(BV)
                vT = Pm[:, 128:256]
                op("vector", "tensor_copy", dict(out=vT, in_=ps), R=qk, W=[Pm])
                q, qk = rq()
                op("tensor", "transpose", dict(out=rg[:, q * 128:(q + 1) * 128], in_=vT, identity=idn[:]), R=[Pm, idn], W=qk)
                op("vector", "tensor_copy", dict(out=VB[:, par, :], in_=rg[:, q * 128:(q + 1) * 128]), R=qk, W=[VB])
            if full:
                Rv = R_[:].rearrange("p (h l) -> p h l", l=128)
                op("vector", "tensor_tensor", dict(out=Rv, in0=bc(tri[:].unsqueeze(1), [128, 16, 128]), in1=bc(dtA.unsqueeze(2), [128, 16, 128]), op=ALU.mult),
                   R=[tri, sm], W=[R_])
                for hq in range(4):
                    b, bk = rb()
                    op("tensor", "matmul", dict(out=rg[:, b * 512:(b + 1) * 512], lhsT=mst[:], rhs=R_[:, hq * 512:(hq + 1) * 512], start=True, stop=True),
                       R=[mst, R_], W=bk)
                    op("scalar", "activation", dict(out=Dm[:, hq * 512:(hq + 1) * 512], in_=rg[:, b * 512:(b + 1) * 512], func=AF.Exp), R=bk, W=[Dm])
                q0, qk0 = rq()
                q1, qk1 = rq()
                cbm = Mm[:, 256:512].rearrange("p (g l) -> p g l", l=128)
                for g, (q, qk) in enumerate(((q0, qk0), (q1, qk1))):
                    op("tensor", "matmul", dict(out=rg[:, q * 128:(q + 1) * 128], lhsT=XAv[:, 8 + g, :], rhs=XAv[:, 10 + g, :], start=True, stop=True),
                       R=[XA], W=qk)
                    op("vector", "tensor_tensor", dict(out=cbm[:, g, :], in0=rg[:, q * 128:(q + 1) * 128], in1=tri[:], op=ALU.mult), R=qk + [tri], W=[Mm])
                Dv = Dm[:].rearrange("p (g r l) -> p g r l", g=2, r=8)
                op("vector", "tensor_tensor", dict(out=Dv, in0=Dv, in1=bc(cbm.unsqueeze(2), [128, 2, 8, 128]), op=ALU.mult), R=[Dm, Mm], W=[Dm])
                Dv3 = Dm[:].rearrange("p (h l) -> p h l", l=128)
                op("vector", "tensor_tensor", dict(out=Dv3, in0=Dv3, in1=bc(dt.unsqueeze(2), [128, 16, 128]), op=ALU.mult), R=[Dm, sm], W=[Dm])
                for h in range(16):
                    g = h // 8
                    op("tensor", "matmul", dict(out=acc[:, h * 64:(h + 1) * 64], lhsT=Dv3[:, h, :], rhs=XS[:, h * 64:(h + 1) * 64], start=True, stop=True),
                       R=[Dm, B5], W=[ACCK[h // 8]])
                for h in range(16):
                    g = h // 8
                    op("tensor", "matmul", dict(out=acc[:, 1024 + h * 64:1024 + (h + 1) * 64], lhsT=XAv[:, 10 + g, :], rhs=S[:, h * 64:(h + 1) * 64], start=True, stop=True),
                       R=[XA, S], W=[ACCK[2 + h // 8]])
                cat = cacc
                catv = cat[:, 0:1024].rearrange("p (h d) -> p h d", d=64)
                tmpv = ctmp[:, 0:1024].rearrange("p (h d) -> p h d", d=64)
                op("vector", "tensor_tensor", dict(out=tmpv, in0=acc[:, 1024:2048].rearrange("p (h d) -> p h d", d=64), in1=bc(ea.unsqueeze(2), [128, 16, 64]), op=ALU.mult),
                   R=[ACCK[2], ACCK[3], sm], W=[ctmp])
                op("vector", "tensor_tensor", dict(out=cat[:, 0:1024], in0=acc[:, 0:1024], in1=ctmp[:, 0:1024], op=ALU.add), R=[ACCK[0], ACCK[1], ctmp], W=[cacc])
                op("vector", "tensor_tensor", dict(out=tmpv, in0=XS.rearrange("p (h d) -> p h d", d=64), in1=bc(dsk_bc[:].unsqueeze(2), [128, 16, 64]), op=ALU.mult),
                   R=[B5, dsk_bc], W=[ctmp])
                op("vector", "tensor_tensor", dict(out=cat[:, 0:1024], in0=cat[:, 0:1024], in1=ctmp[:, 0:1024], op=ALU.add), R=[cacc, ctmp], W=[cacc])
            op("vector", "tensor_tensor", dict(out=XSd.rearrange("p (h d) -> p h d", d=64), in0=XS.rearrange("p (h d) -> p h d", d=64),
                                               in1=bc(dte.unsqueeze(2), [128, 16, 64]), op=ALU.mult), R=[B5, sm], W=[B5])
            b0, bk0 = rb()
            b1, bk1 = rb()
            for h in range(16):
                g = h // 8
                bnk, bk = (b0, bk0) if h < 8 else (b1, bk1)
                o0 = bnk * 512 + (h % 8) * 64
                op("tensor", "matmul", dict(out=rg[:, o0:o0 + 64], lhsT=Btok[:, g * 128:(g + 1) * 128], rhs=XSd[:, h * 64:(h + 1) * 64], start=True, stop=True),
                   R=[Mm, B5], W=bk)
            Sv = S[:].rearrange("p (h d) -> p h d", d=64)
            op("vector", "tensor_tensor", dict(out=Sv, in0=Sv, in1=bc(cd.unsqueeze(2), [128, 16, 64]), op=ALU.mult), R=[S, sm], W=[S])
            op("vector", "tensor_tensor", dict(out=S[:, 0:512], in0=S[:, 0:512], in1=rg[:, b0 * 512:(b0 + 1) * 512], op=ALU.add), R=[S] + bk0, W=[S])
            op("vector", "tensor_tensor", dict(out=S[:, 512:1024], in0=S[:, 512:1024], in1=rg[:, b1 * 512:(b1 + 1) * 512], op=ALU.add), R=[S] + bk1, W=[S])
            if not full:
                return
            for g in range(2):
                for half in range(2):
                    ab = {(0, 0): 0, (0, 1): 1, (1, 0): 1, (1, 1): 0}[(g, half)]
                    pts = []
                    for kb in range(2):
                        kpar = par if kb == 1 else 1 - par
                        b, bk = rb()
                        op("tensor", "matmul", dict(out=rg[:, b * 512:(b + 1) * 512], lhsT=KTb[64 * half:64 * half + 64, ab, kpar, :],
                                                    rhs=qT[64 * half:64 * half + 64, 4 * g:4 * g + 4, :], start=True, stop=True), R=[KTb, qTk], W=bk)
                        (pe, pek), (pm, pmk) = pTe[kb], pTm[kb]
                        op("scalar", "activation", dict(out=pe, in_=rg[:, b * 512:(b + 1) * 512], func=AF.Exp), R=bk, W=[pek])
                        msk = tri if kb == 1 else (mprevF if tau == 0 else mst)
                        op("vector", "tensor_tensor", dict(out=pm.rearrange("p (j t) -> p j t", t=128), in0=pe.rearrange("p (j t) -> p j t", t=128),
                                                           in1=bc(msk[:].unsqueeze(1), [128, 4, 128]), op=ALU.mult), R=[pek, msk], W=[pmk])
                        pts.append((pm, kpar))
                    for j in range(4):
                        hd = 2 * (4 * g + j) + half
                        for kb in range(2):
                            pm, kpar = pts[kb]
                            op("tensor", "matmul", dict(out=acc[:, hd * 64:(hd + 1) * 64], lhsT=pm[:, j * 128:(j + 1) * 128], rhs=VB[:, kpar, g * 64:(g + 1) * 64],
                                                        start=(kb == 0), stop=(kb == 1)), R=[G[1], VB], W=[ACCK[hd // 8]])
                        for kb in range(2):
                            pm, kpar = pts[kb]
                            op("tensor", "matmul", dict(out=acc[:, 1024 + hd:1025 + hd], lhsT=pm[:, j * 128:(j + 1) * 128], rhs=onesb[:, 0:1],
                                                        start=(kb == 0), stop=(kb == 1)), R=[G[1], onesb], W=[ACCK[2]])
            den = sm[:, 112:128]
            op("vector", "tensor_tensor", dict(out=den, in0=acc[:, 1024:1040], in1=esink[:], op=ALU.add), R=[ACCK[2], esink], W=[sm])
            op("vector", "reciprocal", dict(out=den, in_=den), R=[sm], W=[sm])
            op("vector", "tensor_tensor", dict(out=cat[:, 1024:2048].rearrange("p (h d) -> p h d", d=64), in0=acc[:, 0:1024].rearrange("p (h d) -> p h d", d=64),
                                               in1=bc(den.unsqueeze(2), [128, 16, 64]), op=ALU.mult), R=[ACCK[0], ACCK[1], sm], W=[cacc])
            transposes_f32(lambda j: szT[:, j, :], 8, lambda j, c: B6[:, 1024 + j * 128:1024 + (j + c) * 128], [B6], [B6])
            op("vector", "tensor_tensor", dict(out=cat[:, 0:1024], in0=cat[:, 0:1024], in1=sz, op=ALU.mult), R=[cacc, B6], W=[cacc])
            st = sm[:, 128:152]; mv = sm[:, 152:156]; rs = sm[:, 156:158]
            for i in range(4):
                op("vector", "bn_stats", dict(out=st[:, i * 6:(i + 1) * 6], in_=cat[:, i * 512:(i + 1) * 512]), R=[cacc], W=[sm])
            for i in range(2):
                op("vector", "bn_aggr", dict(out=mv[:, 2 * i:2 * i + 2], in_=st[:, i * 12:(i + 1) * 12]), R=[sm], W=[sm])
                op("vector", "tensor_tensor", dict(out=rs[:, i:i + 1], in0=mv[:, 2 * i:2 * i + 1], in1=mv[:, 2 * i:2 * i + 1], op=ALU.mult), R=[sm], W=[sm])
                op("vector", "tensor_tensor", dict(out=rs[:, i:i + 1], in0=rs[:, i:i + 1], in1=mv[:, 2 * i + 1:2 * i + 2], op=ALU.add), R=[sm], W=[sm])
            rstd_from(rs, rs, sm)
            catb = ctmp[:].bitcast(BF16)
            for i in range(2):
                op("vector", "tensor_scalar", dict(out=catb[:, i * 1024:(i + 1) * 1024], in0=cat[:, i * 1024:(i + 1) * 1024], scalar1=rs[:, i:i + 1], scalar2=None, op0=ALU.mult),
                   R=[cacc, sm], W=[ctmp])
            catT = XA[:].bitcast(BF16)[:, 0:2048].rearrange("p (k t) -> p k t", t=128)
            for half in range(2):
                b, bk = rb()
                for k in range(8):
                    kc = half * 8 + k
                    op("tensor", "transpose", dict(out=rgb[:, b * 1024 + k * 128:b * 1024 + (k + 1) * 128], in_=catb[:, kc * 128:(kc + 1) * 128], identity=idb[:]),
                       R=[ctmp, idb], W=bk)
                op("scalar", "copy", dict(out=XA[:].bitcast(BF16)[:, half * 1024:(half + 1) * 1024], in_=rgb[:, b * 1024:(b + 1) * 1024]), R=bk, W=[XA])
            for cbk in range(8):
                w = ld_wout(cbk)
                for kc in range(16):
                    op("tensor", "matmul", dict(out=acc[:, cbk * 256:(cbk + 1) * 256], lhsT=catT[:, kc, :], rhs=w[:, kc, :], start=(kc == 0), stop=(kc == 15)),
                       R=[XA, w], W=[ACCK[cbk // 2]])
            v = vv_
            op("vector", "scalar_tensor_tensor", dict(out=v[:], in0=xtl[:], scalar=ALPHA, in1=acc[:], op0=ALU.mult, op1=ALU.add), R=[xtl] + ACCK, W=[v])
            for i in range(4):
                op("vector", "bn_stats", dict(out=st[:, i * 6:(i + 1) * 6], in_=v[:, i * 512:(i + 1) * 512]), R=[v], W=[sm])
            op("vector", "bn_aggr", dict(out=mv[:, 0:2], in_=st), R=[sm], W=[sm])
            rstd_from(mv[:, 1:2], rs[:, 0:1], sm)
            op("vector", "tensor_scalar", dict(out=v[:], in0=v[:], scalar1=mv[:, 0:1], scalar2=rs[:, 0:1], op0=ALU.subtract, op1=ALU.mult), R=[v, sm], W=[v])

        def peer_tile(tau):
            R_, Dm, cacc, ctmp, XA, B5, B6, x1n = Bs
            for j0 in range(0, 16, 4):
                b, bk = rb()
                for k in range(4):
                    kc = j0 + k
                    op("tensor", "transpose", dict(out=rg[:, b * 512 + k * 128:b * 512 + (k + 1) * 128], in_=x1n[:, kc * 128:(kc + 1) * 128], identity=idn[:]),
                       R=[x1n, idn], W=bk)
                for k in range(4):
                    kc = j0 + k
                    op("scalar", "activation", dict(out=h2T[:, kc, :], in_=rg[:, b * 512 + k * 128:b * 512 + (k + 1) * 128], func=AF.Identity,
                                                    scale=A2[:, kc:kc + 1], bias=B2[:, kc:kc + 1]), R=bk + [A2, B2], W=[h2T])
            QT = R_[:].rearrange("p (a b) -> p a b", b=128)
            for qb in range(16):
                w = ld_wq(qb)
                q, qk = rq()
                for kc in range(16):
                    op("tensor", "matmul", dict(out=rg[:, q * 128:(q + 1) * 128], lhsT=w[:, kc, :], rhs=h2T[:, kc, :], start=(kc == 0), stop=(kc == 15)),
                       R=[w, h2T], W=qk)
                op("vector", "tensor_copy", dict(out=QT[:, qb, :], in_=rg[:, q * 128:(q + 1) * 128]), R=qk, W=[R_])
            SC = Dm[:].rearrange("p (a b) -> p a b", b=128)
            for hc in range(16):
                q, qk = rq()
                op("tensor", "matmul", dict(out=rg[:, q * 128:(q + 1) * 128], lhsT=QT[:, hc, :], rhs=KT[:, hc, :], start=True, stop=True), R=[R_, KT], W=qk)
                op("vector", "tensor_copy", dict(out=SC[:, hc, :], in_=rg[:, q * 128:(q + 1) * 128]), R=qk, W=[Dm])
            for hc in range(16):
                op("vector", "max", dict(out=mx8[:, hc, :], in_=SC[:, hc, :]), R=[Dm], W=[mx8])
            E = cacc[:].rearrange("p (a b) -> p a b", b=128)
            op("vector", "tensor_tensor", dict(out=E, in0=SC, in1=bc(mx8[:, :, 0:1], [128, 16, 128]), op=ALU.subtract), R=[Dm, mx8], W=[cacc])
            op("scalar", "activation", dict(out=cacc[:], in_=cacc[:], func=AF.Exp), R=[cacc], W=[cacc])
            for hc in range(16):
                op("vector", "max", dict(out=Vt[:, hc, 0:8], in_=E[:, hc, :]), R=[cacc], W=[Vt])
                op("vector", "match_replace", dict(out=wk[:, 0:128], in_to_replace=Vt[:, hc, 0:8], in_values=E[:, hc, :], imm_value=-1.0), R=[cacc, Vt], W=[wk])
                op("vector", "max", dict(out=Vt[:, hc, 8:16], in_=wk[:, 0:128]), R=[wk], W=[Vt])
            cand = ctmp[:].rearrange("p (h a b) -> p h a b", h=8, a=16)
            V4 = Vt[:].rearrange("p (h c) k -> p h c k", c=2)
            op("vector", "tensor_tensor", dict(out=cand, in0=bc(V4[:, :, 0, :].unsqueeze(3), [128, 8, 16, 16]), in1=bc(V4[:, :, 1, :].unsqueeze(2), [128, 8, 16, 16]), op=ALU.mult),
               R=[Vt], W=[ctmp])
            candf = ctmp[:].rearrange("p (h n) -> p h n", h=8)
            for h in range(8):
                op("vector", "max", dict(out=BVt[:, h, 0:8], in_=candf[:, h, :]), R=[ctmp], W=[BVt])
                op("vector", "match_replace", dict(out=wk[:], in_to_replace=BVt[:, h, 0:8], in_values=candf[:, h, :], imm_value=-1.0), R=[ctmp, BVt], W=[wk])
                op("vector", "max", dict(out=BVt[:, h, 8:16], in_=wk[:]), R=[wk], W=[BVt])
            th = sm[:, 160:168]; rz = sm[:, 168:176]
            op("vector", "tensor_scalar", dict(out=th, in0=BVt[:, :, 15], scalar1=0.999999, scalar2=None, op0=ALU.mult), R=[BVt], W=[sm])
            op("vector", "tensor_reduce", dict(out=rz, in_=BVt[:], axis=AX.X, op=ALU.add), R=[BVt], W=[sm])
            op("vector", "reciprocal", dict(out=rz, in_=rz), R=[sm], W=[sm])
            E4 = cacc[:].rearrange("p (h c n) -> p h c n", h=8, c=2)
            E0v = E4[:, :, 0, :]
            op("vector", "tensor_tensor", dict(out=E0v, in0=E0v, in1=bc(rz.unsqueeze(2), [128, 8, 128]), op=ALU.mult), R=[cacc, sm], W=[cacc])
            op("vector", "tensor_tensor", dict(out=th, in0=th, in1=rz, op=ALU.mult), R=[sm], W=[sm])
            Pb = [Pm[:, i * 512:(i + 1) * 512] for i in range(2)]
            Mmb = Mm[:].bitcast(BF16); M2b = M2[:]
            Mb = [Mmb[:, i * 512:(i + 1) * 512] for i in range(4)] + [M2b[:, i * 512:(i + 1) * 512] for i in range(4)]
            PK = ["Pk0", "Pk1"]; MK = ["Mk%d" % i for i in range(8)]
            op("vector", "tensor_copy", dict(out=sm[:, 255:256], in_=sm[:, 254:255]), R=[Pm, Mm, sm], W=PK + MK + [sm])
            nstep = [0]
            hc_ = 0
            sc_["ut"] = 0; sc_["vv"] = 0

            def ymm(i2p, vblp):
                for ci in range(4):
                    vblk, vk = vblp[ci // 2]
                    for cg in range(4):
                        op("tensor", "matmul", dict(out=acc[:, cg * 512:(cg + 1) * 512], lhsT=wgT[i2p][:, ci, :], rhs=vblk[:, ci % 2, cg * 512:(cg + 1) * 512],
                                                    start=(nstep[0] == 0), stop=(nstep[0] == 127)), R=[wgT[i2p], vk], W=[ACCK[cg]])
                    nstep[0] += 1

            prev = None
            for gb in range(32):
                gbank, gk = rb()
                for h in range(8):
                    k = hc_ % 2; hc_ += 1
                    op("vector" if h == 5 else "gpsimd", "tensor_tensor", dict(out=Pb[k].rearrange("p (i j) -> p i j", j=128), in0=bc(E4[:, h, 0, gb * 4:(gb + 1) * 4].unsqueeze(2), [128, 4, 128]),
                                                       in1=bc(E4[:, h, 1, :].unsqueeze(1), [128, 4, 128]), op=ALU.mult), R=[cacc], W=[PK[k]])
                    op("vector", "scalar_tensor_tensor", dict(out=Mb[h], in0=Pb[k], scalar=th[:, h:h + 1], in1=Pb[k], op0=ALU.is_ge, op1=ALU.mult), R=[PK[k], sm], W=[MK[h]])
                i2 = gb % 2
                b, bk = rb()
                for sb in range(2):
                    eb = gb * 2 + sb
                    u, uk = ld_ut(eb)
                    for kc in range(16):
                        op("tensor", "matmul", dict(out=rg[:, b * 512 + sb * 256:b * 512 + (sb + 1) * 256], lhsT=h2T[:, kc, :], rhs=u[:, kc, :], start=(kc == 0), stop=(kc == 15)),
                           R=[h2T, uk], W=bk)
                for h in range(8):
                    op("tensor", "matmul", dict(out=rg[:, gbank * 512:(gbank + 1) * 512], lhsT=idb[:], rhs=Mb[h], start=(h == 0), stop=(h == 7)), R=[idb, MK[h]], W=gk)
                op("scalar", "activation", dict(out=gel[i2][:], in_=rg[:, b * 512:(b + 1) * 512], func=AF.Gelu), R=bk, W=[gel[i2]])
                op("vector", "tensor_tensor", dict(out=wg[i2][:], in0=rg[:, gbank * 512:(gbank + 1) * 512], in1=gel[i2][:], op=ALU.mult), R=[gel[i2]] + gk, W=[wg[i2]])
                if prev is not None:
                    ymm(*prev)
                b2, bk2 = rb()
                for ci in range(4):
                    op("tensor", "transpose", dict(out=rgb[:, b2 * 1024 + ci * 128:b2 * 1024 + (ci + 1) * 128], in_=wg[i2][:, ci * 128:(ci + 1) * 128], identity=idb[:]),
                       R=[wg[i2], idb], W=bk2)
                op("scalar", "copy", dict(out=wgT[i2][:].rearrange("p c t -> p (c t)"), in_=rgb[:, b2 * 1024:b2 * 1024 + 512]), R=bk2, W=[wgT[i2]])
                vbl = [ld_vv(gb * 2), ld_vv(gb * 2 + 1)]
                prev = (i2, vbl)
            ymm(*prev)
            op("vector", "tensor_copy", dict(out=sm[:, 255:256], in_=sm[:, 254:255]), R=PK + MK + [sm], W=[Pm, Mm, sm])
            g1t, b1t, g2t, b2t = R_, Dm, XA, B5
            dma(g1t[:], l1g.partition_broadcast(128), W=[g1t], key="bc0")
            dma(b1t[:], l1b.partition_broadcast(128), W=[b1t], key="bc1")
            dma(g2t[:], l2g.partition_broadcast(128), W=[g2t], key="bc2")
            dma(b2t[:], l2b.partition_broadcast(128), W=[b2t], key="bc3")
            v2 = B6
            op("vector", "tensor_tensor", dict(out=v2[:], in0=x1n[:], in1=g1t[:], op=ALU.mult), R=[x1n, g1t], W=[v2])
            op("vector", "tensor_tensor", dict(out=v2[:], in0=v2[:], in1=b1t[:], op=ALU.add), R=[v2, b1t], W=[v2])
            op("vector", "scalar_tensor_tensor", dict(out=v2[:], in0=v2[:], scalar=ALPHA, in1=acc[:], op0=ALU.mult, op1=ALU.add), R=[v2] + ACCK, W=[v2])
            st = sm[:, 128:152]; mv = sm[:, 152:156]; rs = sm[:, 156:158]
            for i in range(4):
                op("vector", "bn_stats", dict(out=st[:, i * 6:(i + 1) * 6], in_=v2[:, i * 512:(i + 1) * 512]), R=[v2], W=[sm])
            op("vector", "bn_aggr", dict(out=mv[:, 0:2], in_=st), R=[sm], W=[sm])
            rstd_from(mv[:, 1:2], rs[:, 0:1], sm)
            op("vector", "tensor_scalar", dict(out=v2[:], in0=v2[:], scalar1=mv[:, 0:1], scalar2=rs[:, 0:1], op0=ALU.subtract, op1=ALU.mult), R=[v2, sm], W=[v2])
            op("vector", "tensor_tensor", dict(out=v2[:], in0=v2[:], in1=g2t[:], op=ALU.mult), R=[v2, g2t], W=[v2])
            op("vector", "tensor_tensor", dict(out=v2[:], in0=v2[:], in1=b2t[:], op=ALU.add), R=[v2, b2t], W=[v2])
            dma(out[tau * T:(tau + 1) * T, :], v2[:], R=[v2], key="o%d" % (tau % 2), q="scalar")

        for tau in range(-npre, nt):
            mixer_tile(tau)
            if tau >= 0:
                if stop == "mix":
                    dma(out[tau * T:(tau + 1) * T, :], Bs[7][:], R=[Bs[7]], key="o%d" % (tau % 2), q="scalar")
                    continue
                peer_tile(tau)
        P.emit(final_wait=["o0", "o1"] if nt > 1 else ["o0"])
    return nc


def host_prep(inp, nt=16, npre=16, ncores=8, seq=4096):
    f = np.float32
    w_in = inp["w_in"][0]
    cols = []
    cols += [w_in[:, Z0:Z0 + 1024], w_in[:, XS0:XS0 + 1536]]
    cols += [w_in[:, Q0:Q0 + 1024]]
    k0 = w_in[:, K0:K0 + 64]; k1 = w_in[:, K0 + 64:K0 + 128]
    cols += [k0, k1, k1, k0, w_in[:, V0:V0 + 128], w_in[:, DT0:DT0 + 16], np.zeros((D, 112), f)]
    wp = np.ascontiguousarray(np.concatenate(cols, axis=1))
    assert wp.shape == (D, 4096)
    cw = np.ascontiguousarray(inp["conv_w"][0].T.reshape(12, 128, 4).transpose(1, 0, 2))
    cb = np.ascontiguousarray(inp["conv_b"][0].reshape(12, 128).T)
    fm = lambda v: np.ascontiguousarray(v.reshape(16, 128).T)
    gcat = fm(np.concatenate([inp["ssm_norm_g"][0], inp["attn_norm_g"][0]]))
    ii = np.arange(128)
    tri = (ii[:, None] <= ii[None, :]).astype(f)
    mst = (ii[:, None] > ii[None, :]).astype(f)
    shared = dict(idn=np.eye(128, dtype=f), tri=tri, mst=mst, w_ada=inp["w_ada"][0], b_ada=inp["b_ada"][0], wp=wp, convw=cw, convb=cb,
                  dt_bias=inp["dt_bias"][0], a_log=inp["a_log"][0], d_skip=inp["d_skip"][0], sinks=inp["attn_sinks"][0], gcat=gcat,
                  w_out=inp["w_out"][0], ln1_g=inp["ln1_g"][0], ln1_b=inp["ln1_b"][0], l1gT=fm(inp["ln1_g"][0]), l1bT=fm(inp["ln1_b"][0]),
                  w_q=inp["peer_w_q"][0], keys=np.ascontiguousarray(inp["peer_sub_keys"][0].reshape(16, 128, 128)),
                  peer_u=inp["peer_u"][0], peer_v=inp["peer_v"][0], ln2_g=inp["ln2_g"][0], ln2_b=inp["ln2_b"][0])
    x = inp["x"]; c = inp["c"]
    per_seq = seq // (nt * T)
    maps = []
    for core in range(ncores):
        b = core // per_seq; part = core % per_seq
        s0 = part * nt * T
        m = dict(shared)
        m["xm"] = np.ascontiguousarray(x[b, s0:s0 + nt * T])
        if part == 0:
            m["xp"] = np.zeros((max(npre, 1) * T, D), f)
            m["flag"] = np.zeros((128, 1), f)
        else:
            m["xp"] = np.ascontiguousarray(x[b, s0 - npre * T:s0])
            m["flag"] = np.ones((128, 1), f)
        m["cT"] = fm(c[b])
        maps.append(m)
    return maps


def kernel(**inputs):
    inp = {k: np.asarray(v) for k, v in inputs.items()}
    nc = build(16, 16)
    maps = host_prep(inp)
    res = run_bass_kernel_spmd(nc, maps, core_ids=list(range(8)))
    outs = [r["out"] for r in res.results]
    y = np.stack(outs, 0).reshape(4, 4096, D).astype(np.float32)
    return y
```

```python
import contextlib
import numpy as np
import concourse.bass as bass
import concourse.mybir as mybir

F32 = mybir.dt.float32
BF16 = mybir.dt.bfloat16
AF = mybir.ActivationFunctionType
ALU = mybir.AluOpType
AX = mybir.AxisListType

ENGS = ("tensor", "vector", "scalar", "gpsimd", "sync")


class Prog:
    def __init__(self, nc):
        self.nc = nc
        self.ops = []
        self.last_w = {}
        self.readers = {}
        self.eng_last = {e: None for e in ENGS}
        self.dma_keys = {}

    def op(self, eng, fn, reads=(), writes=(), dma=None, multi_w=False):
        i = len(self.ops)
        deps = set()
        reads = [r if isinstance(r, (str, tuple)) else r.name for r in reads]
        writes = [w if isinstance(w, (str, tuple)) else w.name for w in writes]
        for r in reads:
            deps.update(self.last_w.get(r, ()))
        for w in writes:
            deps.update(self.last_w.get(w, ()))
            deps.update(self.readers.get(w, ()))
        for r in reads:
            self.readers.setdefault(r, []).append(i)
        for w in writes:
            if multi_w:
                self.last_w.setdefault(w, []).append(i)
            else:
                self.last_w[w] = [i]
            self.readers[w] = []
        deps.discard(i)
        self.ops.append(dict(eng=eng, fn=fn, deps=deps, dma=dma))
        if dma is not None:
            self.dma_keys.setdefault(dma, []).append(i)
        return i

    def barrier(self):
        allprev = set(range(len(self.ops)))
        need = set()
        for e in ENGS:
            for j in range(len(self.ops) - 1, -1, -1):
                if self.ops[j]["eng"] == e and self.ops[j]["dma"] is None:
                    need.add(j)
                    break
        for k, lst in self.dma_keys.items():
            need.add(lst[-1])
        for e in ENGS:
            i = len(self.ops)
            self.ops.append(dict(eng=e, fn=None, deps=set(need), dma=None))
        self.last_w = {}
        self.readers = {}

    def emit(self, final_wait=()):
        nc = self.nc
        ops = self.ops
        sig = [False] * len(ops)
        for i, o in enumerate(ops):
            for d in o["deps"]:
                od = ops[d]
                if od["dma"] is not None:
                    continue
                if od["eng"] == "tensor" and o["eng"] == "tensor" and o["dma"] is None:
                    continue
                sig[d] = True
        cnt = {e: 0 for e in ENGS}
        sigval = {}
        for i, o in enumerate(ops):
            if o["dma"] is not None:
                continue
            if sig[i]:
                cnt[o["eng"]] += 1
                sigval[i] = cnt[o["eng"]]
        dmaval = {}
        for k, lst in self.dma_keys.items():
            for n, i in enumerate(lst):
                dmaval[i] = 16 * (n + 1)
        with contextlib.ExitStack() as st:
            esem = {e: st.enter_context(nc.semaphore("s_" + e)) for e in ENGS}
            dsem = {k: st.enter_context(nc.semaphore("d_%d" % n)) for n, k in enumerate(self.dma_keys)}
            block = st.enter_context(nc.Block())
            per = {e: [i for i, o in enumerate(ops) if o["eng"] == e] for e in ENGS}

            def run(e, eng):
                known = {}
                for i in per[e]:
                    o = ops[i]
                    waits = {}
                    for d in o["deps"]:
                        od = ops[d]
                        if od["dma"] is not None:
                            s, v = dsem[od["dma"]], dmaval[d]
                        else:
                            if od["fn"] is None:
                                continue
                            if od["eng"] == "tensor" and e == "tensor" and o["dma"] is None:
                                continue
                            s, v = esem[od["eng"]], sigval[d]
                        key = id(s)
                        if waits.get(key, (None, 0))[1] < v:
                            waits[key] = (s, v)
                    for key, (s, v) in waits.items():
                        if known.get(key, 0) < v:
                            eng.wait_ge(s, v)
                            known[key] = v
                    if o["fn"] is None:
                        continue
                    ins = o["fn"](eng)
                    if o["dma"] is not None:
                        ins.then_inc(dsem[o["dma"]], 16)
                    elif sig[i]:
                        ins.then_inc(esem[e], 1)
                if e == "sync":
                    for k in final_wait:
                        lst = self.dma_keys[k]
                        eng.wait_ge(dsem[k], 16 * len(lst))

            @block.tensor
            def _(eng):
                run("tensor", eng)

            @block.vector
            def _(eng):
                run("vector", eng)

            @block.scalar
            def _(eng):
                run("scalar", eng)

            @block.gpsimd
            def _(eng):
                run("gpsimd", eng)

            @block.sync
            def _(eng):
                run("sync", eng)


from concourse.bass_utils import run_bass_kernel_spmd

D = 2048
KC = 16
T = 128
ALPHA = 2.0 ** 0.25
EPS = 1e-5
NEB = 64
Z0, XS0, B0_, C0_, DT0, Q0, K0, V0 = 0, 1024, 2048, 2304, 2560, 2576, 3600, 3728
BZ, BXS, BB, BC, BQ, BKA, BKB, BV, BDT = 0, 8, 16, 18, 20, 28, 29, 30, 31


def build(nt=16, npre=16, stop=None):
    nc = bass.Bass("TRN2", target_bir_lowering=False)
    P = Prog(nc)
    ntok = nt * T
    din = lambda n, s, d=F32: nc.dram_tensor(n, list(s), d, kind="ExternalInput").ap()
    xm = din("xm", [ntok, D]); xp = din("xp", [max(npre, 1) * T, D])
    cT = din("cT", [128, 16]); flag = din("flag", [128, 1])
    idn_d = din("idn", [128, 128]); tri_d = din("tri", [128, 128]); mst_d = din("mst", [128, 128])
    w_ada = din("w_ada", [D, 6 * D]); b_ada = din("b_ada", [6 * D])
    wp = din("wp", [D, 4096]); convw = din("convw", [128, 12, 4]); convb = din("convb", [128, 12])
    dtb = din("dt_bias", [16]); alog = din("a_log", [16]); dsk = din("d_skip", [16]); snk = din("sinks", [16])
    gcat = din("gcat", [128, 16]); w_out = din("w_out", [D, D])
    l1g = din("ln1_g", [D]); l1b = din("ln1_b", [D]); l1gT = din("l1gT", [128, 16]); l1bT = din("l1bT", [128, 16])
    w_q = din("w_q", [D, D]); keys = din("keys", [16, 128, 128])
    pu = din("peer_u", [16384, D]); pv = din("peer_v", [16384, D])
    l2g = din("ln2_g", [D]); l2b = din("ln2_b", [D])
    out = nc.dram_tensor("out", [ntok, D], F32, kind="ExternalOutput").ap()
    dint = lambda n, s: nc.dram_tensor(n, list(s), BF16, kind="Internal").ap()
    WIN = dint("WIN", [32, 128, 16, 128]); WOUT = dint("WOUT", [8, 128, 16, 256]); WQ = dint("WQ", [16, 128, 16, 128])
    UT = dint("UT", [NEB, 128, 16, 256]); VV = dint("VV", [NEB, 128, 2, D])

    ctr = [0]

    def nm(p):
        ctr[0] += 1
        return "%s_%d" % (p, ctr[0])

    def op(eng, name, kw, R=(), W=(), **k2):
        return P.op(eng, lambda e: getattr(e, name)(**kw), reads=R, writes=W, **k2)

    def dma(outap, inap, R=(), W=(), key=None, q="sync", **k2):
        return P.op(q, lambda e: e.dma_start(out=outap, in_=inap), reads=R, writes=W, dma=key, **k2)

    def bc(ap, shape):
        return ap.broadcast_to(list(shape))

    with contextlib.ExitStack() as top:
        TT_ = lambda n, s, d=F32: top.enter_context(nc.sbuf_tensor("sb_" + n, list(s), d))
        acc = top.enter_context(nc.psum_tensor("acc", [128, 2048], F32))
        rg = top.enter_context(nc.psum_tensor("rg", [128, 2048], F32))
        rgb = rg[:].bitcast(BF16)
        rstate = dict(n=0)

        def rq():
            n = rstate["n"]; rstate["n"] = n + 1
            b = n % 4; qq = (n // 4) % 4
            return b * 4 + qq, [("rg", b)]

        def rb():
            n = rstate["n"]; rstate["n"] = n + 1
            b = n % 4
            return b, [("rg", b)]

        ACCK = [("acc", j) for j in range(4)]
        idn = TT_("idn", [128, 128]); tri = TT_("tri", [128, 128]); mst = TT_("mst", [128, 128])
        idb = TT_("idb", [128, 128], BF16); mprevF = TT_("mprevF", [128, 128]); ones = TT_("ones", [128, 128])
        onesb = TT_("onesb", [128, 1], BF16); flg = TT_("flg", [128, 1])
        modT = TT_("modT", [128, 64])
        A2 = TT_("A2", [128, 16]); B2 = TT_("B2", [128, 16])
        KT = TT_("KT", [128, 16, 128])
        cw = TT_("cw", [128, 12, 4]); cb = TT_("cb", [128, 12])
        dtb_bc = TT_("dtb_bc", [128, 16]); A_bc = TT_("A_bc", [128, 16]); dsk_bc = TT_("dsk_bc", [128, 16])
        esink = TT_("esink", [128, 16])
        dma(idn[:], idn_d, W=[idn], key="c0"); dma(tri[:], tri_d, W=[tri], key="c1"); dma(mst[:], mst_d, W=[mst], key="c2")
        dma(flg[:], flag, W=[flg], key="c3"); dma(cw[:], convw, W=[cw], key="c4"); dma(cb[:], convb, W=[cb], key="c5")
        dma(dtb_bc[:], dtb.partition_broadcast(128), W=[dtb_bc], key="c6")
        dma(A_bc[:], alog.partition_broadcast(128), W=[A_bc], key="c7")
        dma(dsk_bc[:], dsk.partition_broadcast(128), W=[dsk_bc], key="c8")
        dma(esink[:], snk.partition_broadcast(128), W=[esink], key="c9")
        op("vector", "tensor_copy", dict(out=idb[:], in_=idn[:]), R=[idn], W=[idb])
        op("vector", "memset", dict(ap=ones[:], constant=1.0), W=[ones])
        op("vector", "memset", dict(ap=onesb[:], constant=1.0), W=[onesb])
        op("vector", "tensor_scalar", dict(out=mprevF[:], in0=mst[:], scalar1=flg[:, 0:1], scalar2=None, op0=ALU.mult), R=[mst, flg], W=[mprevF])
        op("scalar", "activation", dict(out=A_bc[:], in_=A_bc[:], func=AF.Exp), R=[A_bc], W=[A_bc])
        op("vector", "tensor_scalar", dict(out=A_bc[:], in0=A_bc[:], scalar1=-1.0, scalar2=None, op0=ALU.mult), R=[A_bc], W=[A_bc])
        op("scalar", "activation", dict(out=esink[:], in_=esink[:], func=AF.Exp), R=[esink], W=[esink])

        with contextlib.ExitStack() as pro:
            PT = lambda n, s, d=F32: pro.enter_context(nc.sbuf_tensor("sp_" + n, list(s), d))
            cTt = PT("cTt", [128, 16]); SCb = PT("SCb", [128, 16, 128])
            g1bc = PT("g1bc", [128, D]); g2bc = PT("g2bc", [128, D]); gct = PT("gct", [128, 16])
            wab = [PT("wab%d" % i, [128, 4, 512]) for i in range(3)]
            bab = [PT("bab%d" % i, [128, 512]) for i in range(2)]
            mrow = [PT("mrow%d" % i, [128, 512]) for i in range(2)]
            st32 = [PT("st32_%d" % i, [128, 4096]) for i in range(2)]
            st16 = [PT("st16_%d" % i, [128, 4096], BF16) for i in range(2)]
            ut16 = [PT("ut16_%d" % i, [128, 4096], BF16) for i in range(2)]
            l1gt = PT("l1gt", [128, 16]); l1bt = PT("l1bt", [128, 16])
            dma(cTt[:], cT, W=[cTt], key="p0"); dma(gct[:], gcat, W=[gct], key="p1")
            dma(l1gt[:], l1gT, W=[l1gt], key="p2"); dma(l1bt[:], l1bT, W=[l1bt], key="p3")
            op("scalar", "activation", dict(out=cTt[:], in_=cTt[:], func=AF.Silu), R=[cTt], W=[cTt])
            op("vector", "tensor_copy", dict(out=SCb[:], in_=bc(cTt[:].unsqueeze(2), [128, 16, 128])), R=[cTt], W=[SCb])

            def finish_early():
                dma(out[0:128, 0:64], modT[:], R=[modT], key="o0", q="scalar")
                dma(out[0:128, 64:80], A2[:], R=[A2], key="o0", q="scalar")
                dma(out[0:128, 80:96], B2[:], R=[B2], key="o0", q="scalar")
                dma(out[0:128, 128:256], KT[:, 3, :], R=[KT], key="o0", q="scalar")
                P.barrier()
                P.emit(final_wait=["o0"])
                return nc
            if stop == "pA":
                return finish_early()
            for hc in range(16):
                kb_ = st32[hc % 2]
                dma(kb_[:, 0:128], keys[hc], W=[kb_], key="pk%d" % (hc % 2))
                q, qk = rq()
                op("tensor", "transpose", dict(out=rg[:, q * 128:(q + 1) * 128], in_=kb_[:, 0:128], identity=idn[:]), R=[kb_, idn], W=qk)
                op("vector", "tensor_copy", dict(out=KT[:, hc, :], in_=rg[:, q * 128:(q + 1) * 128]), R=qk, W=[KT])
            if stop == "p0":
                return finish_early()
            for g in range(24):
                b, bk = rb()
                bb = bab[g % 2]
                dma(bb[:], b_ada[g * 512:(g + 1) * 512].partition_broadcast(128), W=[bb], key="pb%d" % (g % 2))
                for kq in range(4):
                    wb = wab[(g * 4 + kq) % 3]
                    dma(wb[:], w_ada[kq * 512:(kq + 1) * 512, g * 512:(g + 1) * 512].rearrange("(k p) c -> p k c", p=128),
                        W=[wb], key="pw%d" % ((g * 4 + kq) % 3))
                    for k in range(4):
                        kc = kq * 4 + k
                        op("tensor", "matmul", dict(out=rg[:, b * 512:(b + 1) * 512], lhsT=SCb[:, kc, :], rhs=wb[:, k, :],
                                                    start=(kc == 0), stop=(kc == 15)), R=[SCb, wb], W=bk)
                gi = g // 4
                if gi == 2:
                    dst, dk = g1bc[:, (g % 4) * 512:(g % 4 + 1) * 512], g1bc
                elif gi == 5:
                    dst, dk = g2bc[:, (g % 4) * 512:(g % 4 + 1) * 512], g2bc
                else:
                    mr = mrow[g % 2]
                    dst, dk = mr[:], mr
                op("vector", "tensor_tensor", dict(out=dst, in0=rg[:, b * 512:(b + 1) * 512], in1=bb[:], op=ALU.add), R=bk + [bb], W=[dk])
                if gi in (0, 1, 3, 4):
                    col0 = {0: 0, 1: 16, 3: 32, 4: 48}[gi] + (g % 4) * 4
                    b2, bk2 = rb()
                    for j in range(4):
                        op("tensor", "transpose", dict(out=rg[:, b2 * 512 + j * 128: b2 * 512 + (j + 1) * 128], in_=mr[:, j * 128:(j + 1) * 128], identity=idn[:]),
                           R=[mr, idn], W=bk2)
                    src = rg[:, b2 * 512:(b2 + 1) * 512].rearrange("p (j t) -> p j t", t=128)[:, :, 0]
                    if gi in (1, 4):
                        op("vector", "tensor_scalar", dict(out=modT[:, col0:col0 + 4], in0=src, scalar1=1.0, scalar2=None, op0=ALU.add), R=bk2, W=[modT])
                    else:
                        op("vector", "tensor_copy", dict(out=modT[:, col0:col0 + 4], in_=src), R=bk2, W=[modT])
            op("vector", "tensor_tensor", dict(out=A2[:], in0=l1gt[:], in1=modT[:, 48:64], op=ALU.mult), R=[l1gt, modT], W=[A2])
            op("vector", "tensor_tensor", dict(out=B2[:], in0=l1bt[:], in1=modT[:, 48:64], op=ALU.mult), R=[l1bt, modT], W=[B2])
            op("vector", "tensor_tensor", dict(out=B2[:], in0=B2[:], in1=modT[:, 32:48], op=ALU.add), R=[B2, modT], W=[B2])
            if stop == "p1":
                return finish_early()
            pc = [0]

            def stage(src_ap, shape3, mul_bc=None, mul_pp=None, dst=None, dkey=None):
                i = pc[0] % 2; pc[0] += 1
                s32, s16 = st32[i], st16[i]
                n = shape3[1] * shape3[2]
                v32 = s32[:, 0:n].rearrange("p (a b) -> p a b", b=shape3[2])
                v16 = s16[:, 0:n].rearrange("p (a b) -> p a b", b=shape3[2])
                dma(v32, src_ap, W=[s32], key="pl%d" % i)
                if mul_bc is not None:
                    op("vector", "tensor_tensor", dict(out=v32, in0=v32, in1=mul_bc, op=ALU.mult), R=[s32, g1bc, g2bc], W=[s32])
                if mul_pp is not None:
                    op("gpsimd", "tensor_tensor", dict(out=v16, in0=v32, in1=mul_pp, op=ALU.mult), R=[s32, gct], W=[s16])
                else:
                    op("scalar", "copy", dict(out=v16, in_=v32), R=[s32], W=[s16])
                if dst is not None:
                    dma(dst, v16, R=[s16], W=[dkey], key="ps%d" % i, q="scalar", multi_w=True)
                return s16, v16

            for b in range(32):
                stage(wp[:, b * 128:(b + 1) * 128].rearrange("(k p) c -> p k c", p=128), [128, 16, 128], dst=WIN[b], dkey="WIN")
            if stop == "p2":
                return finish_early()
            for b in range(16):
                stage(w_q[:, b * 128:(b + 1) * 128].rearrange("(k p) c -> p k c", p=128), [128, 16, 128], dst=WQ[b], dkey="WQ")
            for b in range(8):
                stage(w_out[:, b * 256:(b + 1) * 256].rearrange("(k p) c -> p k c", p=128), [128, 16, 256],
                      mul_bc=bc(g1bc[:, b * 256:(b + 1) * 256].unsqueeze(1), [128, 16, 256]),
                      mul_pp=bc(gct[:].unsqueeze(2), [128, 16, 256]), dst=WOUT[b], dkey="WOUT")
            if stop == "p3":
                return finish_early()
            for b in range(NEB):
                stage(pv[b * 256:(b + 1) * 256, :].rearrange("(c p) d -> p c d", p=128), [128, 2, D],
                      mul_bc=bc(g2bc[:].unsqueeze(1), [128, 2, D]), dst=VV[b], dkey="VV")
            if stop == "p4":
                return finish_early()
            for b in range(NEB):
                s16, v16 = stage(pu[b * 256:(b + 1) * 256, :].rearrange("(c p) d -> p c d", p=128), [128, 2, D])
                u16 = ut16[b % 2]
                uv = u16[:].rearrange("p (k e) -> p k e", e=256)
                for kg in range(4):
                    bnk, bk = rb()
                    for k in range(4):
                        kc = kg * 4 + k
                        for ci in range(2):
                            o0 = bnk * 1024 + k * 256 + ci * 128
                            op("tensor", "transpose", dict(out=rgb[:, o0:o0 + 128], in_=v16[:, ci, kc * 128:(kc + 1) * 128], identity=idb[:]),
                               R=[s16, idb], W=bk)
                    op("vector", "tensor_copy", dict(out=u16[:, kg * 1024:(kg + 1) * 1024], in_=rgb[:, bnk * 1024:(bnk + 1) * 1024]), R=bk, W=[u16])
                dma(UT[b], uv, R=[u16], W=["UT"], key="pu%d" % (b % 2), q="scalar", multi_w=True)
            P.barrier()
            if stop == "pro":
                dma(out[0:128, 0:64], modT[:], R=[modT], key="o0", q="scalar")
                dma(out[0:128, 64:80], A2[:], R=[A2], key="o0", q="scalar")
                dma(out[0:128, 80:96], B2[:], R=[B2], key="o0", q="scalar")
                P.emit(final_wait=["o0"])
                return nc
        MT = TT_
        xt = [MT("xt%d" % i, [128, D]) for i in range(1)]
        win_r = [MT("winr%d" % i, [128, 16, 128], BF16) for i in range(3)]
        wout_r = [MT("woutr%d" % i, [128, 16, 256], BF16) for i in range(2)]
        wq_r = [MT("wqr%d" % i, [128, 16, 128], BF16) for i in range(3)]
        ut_r = [MT("utr%d" % i, [128, 16, 256], BF16) for i in range(2)]
        vv_r = [MT("vvr%d" % i, [128, 2, D], BF16) for i in range(2)]
        hT = MT("hT", [128, 16, 128], BF16); h2T = hT
        XC = MT("XC", [128, 12, 131]); S = MT("S", [128, 1024])
        KTb = MT("KTb", [128, 2, 2, 128], BF16)
        VB = MT("VB", [128, 2, 128], BF16)
        Bs = [MT("BIG%d" % i, [128, D]) for i in range(8)]
        sm = MT("sm", [128, 256])
        Vt = MT("Vt", [128, 16, 16]); BVt = MT("BVt", [128, 8, 16]); wk = MT("wk", [128, 256])
        mx8 = MT("mx8", [128, 16, 8])
        G = [MT("G0", [128, 1024])] * 2
        Pm = MT("Pm", [128, 1024]); Mm = MT("Mm", [128, 1024]); M2 = MT("M2", [128, 2048], BF16)
        gel = [MT("gel%d" % i, [128, 512]) for i in range(2)]
        wg = [MT("wg%d" % i, [128, 512], BF16) for i in range(2)]
        wgT = [MT("wgT%d" % i, [128, 4, 128], BF16) for i in range(2)]
        G0b = G[0][:].bitcast(BF16); G1b = G0b[:, 1024:2048]
        qT = G0b[:, 0:1024].rearrange("p (a b) -> p a b", b=128); qTk = G[0]
        pTm = [(G1b[:, 0:512], G[1]), (G1b[:, 512:1024], G[1])]
        pTe = [(Pm[:, 512:1024], Pm), (Mm[:, 512:1024], Mm)]
        fz = MT("fz", [128, 2])
        op("vector", "memset", dict(ap=fz[:], constant=0.0))
        op("vector", "memset", dict(ap=sm[:], constant=0.0), W=[sm])
        HTK = [("hT", kc) for kc in range(16)]
        XCK = [("XC", j) for j in range(12)]

        def fence(R, W):
            op("vector", "tensor_copy", dict(out=fz[:, 1:2], in_=fz[:, 0:1]), R=R, W=W)
        op("vector", "memset", dict(ap=XC[:], constant=0.0), W=XCK)
        op("vector", "memset", dict(ap=S[:], constant=0.0), W=[S])
        op("vector", "memset", dict(ap=KTb[:], constant=0.0), W=[KTb])
        op("vector", "memset", dict(ap=VB[:], constant=0.0), W=[VB])

        sc_ = dict(win=0, wout=0, wq=0, ut=0, vv=0, x=0)

        def ld_win(b):
            i = sc_["win"] % 3; sc_["win"] += 1
            dma(win_r[i][:], WIN[b], R=["WIN"], W=[win_r[i]], key="win%d" % i)
            return win_r[i]

        def ld_wout(b):
            i = sc_["wout"] % 2; sc_["wout"] += 1
            dma(wout_r[i][:], WOUT[b], R=["WOUT"], W=[wout_r[i]], key="wout%d" % i)
            return wout_r[i]

        def ld_wq(b):
            i = sc_["wq"] % 3; sc_["wq"] += 1
            dma(wq_r[i][:], WQ[b], R=["WQ"], W=[wq_r[i]], key="wq%d" % i)
            return wq_r[i]

        def _bf(t):
            return t[:].bitcast(BF16)
        ut_slots = [(ut_r[0][:], ut_r[0]), (ut_r[1][:], ut_r[1])] + \
                   [(_bf(Bs[i]).rearrange("p (k e) -> p k e", e=256), Bs[i]) for i in (1, 3, 4)]
        vv_slots = [(vv_r[0][:], vv_r[0]), (vv_r[1][:], vv_r[1])] + \
                   [(_bf(Bs[i]).rearrange("p (c d) -> p c d", d=D), Bs[i]) for i in (5, 6, 0)]

        def ld_ut(b):
            i = sc_["ut"] % 5; sc_["ut"] += 1
            view, kt = ut_slots[i]
            dma(view, UT[b], R=["UT"], W=[kt], key="ut%d" % i)
            return view, kt

        def ld_vv(b):
            i = sc_["vv"] % 5; sc_["vv"] += 1
            view, kt = vv_slots[i]
            dma(view, VV[b], R=["VV"], W=[kt], key="vv%d" % i)
            return view, kt

        def rstd_from(var_ap, out_ap, keyt):
            op("vector", "tensor_scalar", dict(out=out_ap, in0=var_ap, scalar1=EPS, scalar2=None, op0=ALU.add), R=[keyt], W=[keyt])
            op("scalar", "activation", dict(out=out_ap, in_=out_ap, func=AF.Ln), R=[keyt], W=[keyt])
            op("scalar", "activation", dict(out=out_ap, in_=out_ap, func=AF.Exp, scale=-0.5), R=[keyt], W=[keyt])

        def transposes_f32(src_fn, n, dst_fn, dkeys, rkeys, evac="vector"):
            j = 0
            while j < n:
                cnt = min(4, n - j)
                b, bk = rb()
                for k in range(cnt):
                    op("tensor", "transpose", dict(out=rg[:, b * 512 + k * 128: b * 512 + (k + 1) * 128], in_=src_fn(j + k), identity=idn[:]),
                       R=rkeys + [idn], W=bk)
                if evac == "vector":
                    op("vector", "tensor_copy", dict(out=dst_fn(j, cnt), in_=rg[:, b * 512: b * 512 + cnt * 128]), R=bk, W=dkeys)
                else:
                    op("scalar", "copy", dict(out=dst_fn(j, cnt), in_=rg[:, b * 512: b * 512 + cnt * 128]), R=bk, W=dkeys)
                j += cnt

        def mixer_tile(tau):
            full = tau >= 0
            par = tau % 2
            xsrc = xm[tau * T:(tau + 1) * T, :] if full else xp[(npre + tau) * T:(npre + tau + 1) * T, :]
            xi = 0
            xtl = xt[xi]
            dma(xtl[:], xsrc, W=[xtl], key="x%d" % xi)
            if tau == 0:
                op("vector", "tensor_scalar", dict(out=S[:], in0=S[:], scalar1=flg[:, 0:1], scalar2=None, op0=ALU.mult), R=[S, flg], W=[S])
                op("vector", "tensor_scalar", dict(out=XC[:], in0=XC[:], scalar1=flg[:, 0:1], scalar2=None, op0=ALU.mult), R=XCK + [flg], W=XCK)
            for j0 in range(0, 16, 4):
                b, bk = rb()
                for k in range(4):
                    kc = j0 + k
                    op("tensor", "transpose", dict(out=rg[:, b * 512 + k * 128:b * 512 + (k + 1) * 128], in_=xtl[:, kc * 128:(kc + 1) * 128], identity=idn[:]),
                       R=[xtl, idn], W=bk)
                for k in range(4):
                    kc = j0 + k
                    op("scalar", "activation", dict(out=hT[:, kc, :], in_=rg[:, b * 512 + k * 128:b * 512 + (k + 1) * 128], func=AF.Identity,
                                                    scale=modT[:, 16 + kc:17 + kc], bias=modT[:, kc:kc + 1]), R=bk + [modT], W=[HTK[kc]])
            R_, Dm, cacc, ctmp, XA, B5, B6, vv_ = Bs
            XS = B5[:, 0:1024]; XSd = B5[:, 1024:2048]
            szT = B6[:, 0:1024].rearrange("p (a b) -> p a b", b=128); sz = B6[:, 1024:2048]
            XAv = XA[:, 0:1536].rearrange("p (a b) -> p a b", b=128)

            def proj(blk):
                w = ld_win(blk)
                q, qk = rq()
                for kc in range(16):
                    op("tensor", "matmul", dict(out=rg[:, q * 128:(q + 1) * 128], lhsT=w[:, kc, :], rhs=hT[:, kc, :], start=(kc == 0), stop=(kc == 15)),
                       R=[w, HTK[kc]], W=qk)
                return rg[:, q * 128:(q + 1) * 128], qk

            nconv = 12 if (full or tau == -1) else 10
            for j in range(nconv):
                ps, qk = proj(BXS + j)
                op("vector", "tensor_copy", dict(out=XC[:, j, 3:131], in_=ps), R=qk, W=[XCK[j]])
            ca = cacc[:, 0:1536].rearrange("p (a b) -> p a b", b=128)[:, 0:nconv, :]
            ct = ctmp[:, 0:1536].rearrange("p (a b) -> p a b", b=128)[:, 0:nconv, :]
            for k in range(4):
                dst, dk = (ca, cacc) if k == 0 else (ct, ctmp)
                op("vector", "tensor_tensor", dict(out=dst, in0=XC[:, 0:nconv, k:k + 128], in1=bc(cw[:, 0:nconv, k:k + 1], [128, nconv, 128]), op=ALU.mult),
                   R=XCK[0:nconv] + [cw], W=[dk])
                if k > 0:
                    op("vector", "tensor_tensor", dict(out=ca, in0=ca, in1=ct, op=ALU.add), R=[cacc, ctmp], W=[cacc])
            op("vector", "tensor_tensor", dict(out=ca, in0=ca, in1=bc(cb[:, 0:nconv].unsqueeze(2), [128, nconv, 128]), op=ALU.add), R=[cacc, cb], W=[cacc])
            op("scalar", "activation", dict(out=XAv[:, 0:nconv, :], in_=ca, func=AF.Silu), R=[cacc], W=[XA])
            op("vector", "tensor_copy", dict(out=XC[:, :, 0:3], in_=XC[:, :, 128:131]), R=XCK, W=XCK)
            transposes_f32(lambda j: XAv[:, j, :], 8, lambda j, c: B5[:, j * 128:(j + c) * 128], [B5], [XA])
            Btok = sm[:, 0:256]
            Btok = Mm[:, 0:256]
            transposes_f32(lambda j: XAv[:, 8 + j, :], 2, lambda j, c: Mm[:, j * 128:(j + c) * 128], [Mm], [XA])
            ps, qk = proj(BDT)
            dtT = Pm[:, 0:128]
            op("vector", "tensor_copy", dict(out=dtT, in_=ps), R=qk, W=[Pm])
            q, qk = rq()
            op("tensor", "transpose", dict(out=rg[:, q * 128:(q + 1) * 128], in_=dtT, identity=idn[:]), R=[Pm, idn], W=qk)
            dt = sm[:, 0:16]; dtA = sm[:, 16:32]; acum = sm[:, 32:48]; alast = sm[:, 48:64]; dte = sm[:, 64:80]; ea = sm[:, 80:96]; cd = sm[:, 96:112]
            op("vector", "tensor_tensor", dict(out=dt, in0=rg[:, q * 128:q * 128 + 16], in1=dtb_bc[:], op=ALU.add), R=qk + [dtb_bc], W=[sm])
            op("scalar", "activation", dict(out=dt, in_=dt, func=AF.Exp), R=[sm], W=[sm])
            op("vector", "tensor_scalar", dict(out=dt, in0=dt, scalar1=1.0, scalar2=None, op0=ALU.add), R=[sm], W=[sm])
            op("scalar", "activation", dict(out=dt, in_=dt, func=AF.Ln), R=[sm], W=[sm])
            op("vector", "tensor_tensor", dict(out=dtA, in0=dt, in1=A_bc[:], op=ALU.mult), R=[sm, A_bc], W=[sm])
            q, qk = rq()
            op("tensor", "matmul", dict(out=rg[:, q * 128:q * 128 + 16], lhsT=tri[:], rhs=dtA, start=True, stop=True), R=[tri, sm], W=qk)
            op("tensor", "matmul", dict(out=rg[:, q * 128 + 16:q * 128 + 32], lhsT=ones[:], rhs=dtA, start=True, stop=True), R=[ones, sm], W=qk)
            op("vector", "tensor_copy", dict(out=sm[:, 32:64], in_=rg[:, q * 128:q * 128 + 32]), R=qk, W=[sm])
            op("vector", "tensor_tensor", dict(out=dte, in0=alast, in1=acum, op=ALU.subtract), R=[sm], W=[sm])
            op("scalar", "activation", dict(out=dte, in_=dte, func=AF.Exp), R=[sm], W=[sm])
            op("vector", "tensor_tensor", dict(out=dte, in0=dte, in1=dt, op=ALU.mult), R=[sm], W=[sm])
            op("scalar", "activation", dict(out=ea, in_=acum, func=AF.Exp), R=[sm], W=[sm])
            op("scalar", "activation", dict(out=cd, in_=alast, func=AF.Exp), R=[sm], W=[sm])
            if full:
                for j in range(8):
                    ps, qk = proj(BZ + j)
                    op("scalar", "activation", dict(out=szT[:, j, :], in_=ps, func=AF.Silu), R=qk, W=[B6])
                for j in range(8):
                    ps, qk = proj(BQ + j)
                    op("scalar", "activation", dict(out=qT[:, j, :], in_=ps, func=AF.Identity, scale=0.125), R=qk, W=[qTk])
            if full or tau == -1:
                for ab, blk in ((0, BKA), (1, BKB)):
                    ps, qk = proj(blk)
                    op("vector", "tensor_copy", dict(out=KTb[:, ab, par, :], in_=ps), R=qk, W=[KTb])
                ps, qk = proj(BV)
                vT = Pm[:, 128:256]
                op("vector", "tensor_copy", dict(out=vT, in_=ps), R=qk, W=[Pm])
                q, qk = rq()
                op("tensor", "transpose", dict(out=rg[:, q * 128:(q + 1) * 128], in_=vT, identity=idn[:]), R=[Pm, idn], W=qk)
                op("vector", "tensor_copy", dict(out=VB[:, par, :], in_=rg[:, q * 128:(q + 1) * 128]), R=qk, W=[VB])
            if full:
                Rv = R_[:].rearrange("p (h l) -> p h l", l=128)
                op("vector", "tensor_tensor", dict(out=Rv, in0=bc(tri[:].unsqueeze(1), [128, 16, 128]), in1=bc(dtA.unsqueeze(2), [128, 16, 128]), op=ALU.mult),
                   R=[tri, sm], W=[R_])
                for hq in range(4):
                    b, bk = rb()
                    op("tensor", "matmul", dict(out=rg[:, b * 512:(b + 1) * 512], lhsT=mst[:], rhs=R_[:, hq * 512:(hq + 1) * 512], start=True, stop=True),
                       R=[mst, R_], W=bk)
                    op("scalar", "activation", dict(out=Dm[:, hq * 512:(hq + 1) * 512], in_=rg[:, b * 512:(b + 1) * 512], func=AF.Exp), R=bk, W=[Dm])
                q0, qk0 = rq()
                q1, qk1 = rq()
                cbm = Mm[:, 256:512].rearrange("p (g l) -> p g l", l=128)
                for g, (q, qk) in enumerate(((q0, qk0), (q1, qk1))):
                    op("tensor", "matmul", dict(out=rg[:, q * 128:(q + 1) * 128], lhsT=XAv[:, 8 + g, :], rhs=XAv[:, 10 + g, :], start=True, stop=True),
                       R=[XA], W=qk)
                    op("vector", "tensor_tensor", dict(out=cbm[:, g, :], in0=rg[:, q * 128:(q + 1) * 128], in1=tri[:], op=ALU.mult), R=qk + [tri], W=[Mm])
                Dv = Dm[:].rearrange("p (g r l) -> p g r l", g=2, r=8)
                op("vector", "tensor_tensor", dict(out=Dv, in0=Dv, in1=bc(cbm.unsqueeze(2), [128, 2, 8, 128]), op=ALU.mult), R=[Dm, Mm], W=[Dm])
                Dv3 = Dm[:].rearrange("p (h l) -> p h l", l=128)
                op("vector", "tensor_tensor", dict(out=Dv3, in0=Dv3, in1=bc(dt.unsqueeze(2), [128, 16, 128]), op=ALU.mult), R=[Dm, sm], W=[Dm])
                for h in range(16):
                    g = h // 8
                    op("tensor", "matmul", dict(out=acc[:, h * 64:(h + 1) * 64], lhsT=Dv3[:, h, :], rhs=XS[:, h * 64:(h + 1) * 64], start=True, stop=True),
                       R=[Dm, B5], W=[ACCK[h // 8]])
                for h in range(16):
                    g = h // 8
                    op("tensor", "matmul", dict(out=acc[:, 1024 + h * 64:1024 + (h + 1) * 64], lhsT=XAv[:, 10 + g, :], rhs=S[:, h * 64:(h + 1) * 64], start=True, stop=True),
                       R=[XA, S], W=[ACCK[2 + h // 8]])
                cat = cacc
                catv = cat[:, 0:1024].rearrange("p (h d) -> p h d", d=64)
                tmpv = ctmp[:, 0:1024].rearrange("p (h d) -> p h d", d=64)
                op("vector", "tensor_tensor", dict(out=tmpv, in0=acc[:, 1024:2048].rearrange("p (h d) -> p h d", d=64), in1=bc(ea.unsqueeze(2), [128, 16, 64]), op=ALU.mult),
                   R=[ACCK[2], ACCK[3], sm], W=[ctmp])
                op("vector", "tensor_tensor", dict(out=cat[:, 0:1024], in0=acc[:, 0:1024], in1=ctmp[:, 0:1024], op=ALU.add), R=[ACCK[0], ACCK[1], ctmp], W=[cacc])
                op("vector", "tensor_tensor", dict(out=tmpv, in0=XS.rearrange("p (h d) -> p h d", d=64), in1=bc(dsk_bc[:].unsqueeze(2), [128, 16, 64]), op=ALU.mult),
                   R=[B5, dsk_bc], W=[ctmp])
                op("vector", "tensor_tensor", dict(out=cat[:, 0:1024], in0=cat[:, 0:1024], in1=ctmp[:, 0:1024], op=ALU.add), R=[cacc, ctmp], W=[cacc])
            op("vector", "tensor_tensor", dict(out=XSd.rearrange("p (h d) -> p h d", d=64), in0=XS.rearrange("p (h d) -> p h d", d=64),
                                               in1=bc(dte.unsqueeze(2), [128, 16, 64]), op=ALU.mult), R=[B5, sm], W=[B5])
            b0, bk0 = rb()
            b1, bk1 = rb()
            for h in range(16):
                g = h // 8
                bnk, bk = (b0, bk0) if h < 8 else (b1, bk1)
                o0 = bnk * 512 + (h % 8) * 64
                op("tensor", "matmul", dict(out=rg[:, o0:o0 + 64], lhsT=Btok[:, g * 128:(g + 1) * 128], rhs=XSd[:, h * 64:(h + 1) * 64], start=True, stop=True),
                   R=[Mm, B5], W=bk)
            Sv = S[:].rearrange("p (h d) -> p h d", d=64)
            op("vector", "tensor_tensor", dict(out=Sv, in0=Sv, in1=bc(cd.unsqueeze(2), [128, 16, 64]), op=ALU.mult), R=[S, sm], W=[S])
            op("vector", "tensor_tensor", dict(out=S[:, 0:512], in0=S[:, 0:512], in1=rg[:, b0 * 512:(b0 + 1) * 512], op=ALU.add), R=[S] + bk0, W=[S])
            op("vector", "tensor_tensor", dict(out=S[:, 512:1024], in0=S[:, 512:1024], in1=rg[:, b1 * 512:(b1 + 1) * 512], op=ALU.add), R=[S] + bk1, W=[S])
            if not full:
                return
            for g in range(2):
                for half in range(2):
                    ab = {(0, 0): 0, (0, 1): 1, (1, 0): 1, (1, 1): 0}[(g, half)]
                    pts = []
                    for kb in range(2):
                        kpar = par if kb == 1 else 1 - par
                        b, bk = rb()
                        op("tensor", "matmul", dict(out=rg[:, b * 512:(b + 1) * 512], lhsT=KTb[64 * half:64 * half + 64, ab, kpar, :],
                                                    rhs=qT[64 * half:64 * half + 64, 4 * g:4 * g + 4, :], start=True, stop=True), R=[KTb, qTk], W=bk)
                        (pe, pek), (pm, pmk) = pTe[kb], pTm[kb]
                        op("scalar", "activation", dict(out=pe, in_=rg[:, b * 512:(b + 1) * 512], func=AF.Exp), R=bk, W=[pek])
                        msk = tri if kb == 1 else (mprevF if tau == 0 else mst)
                        op("vector", "tensor_tensor", dict(out=pm.rearrange("p (j t) -> p j t", t=128), in0=pe.rearrange("p (j t) -> p j t", t=128),
                                                           in1=bc(msk[:].unsqueeze(1), [128, 4, 128]), op=ALU.mult), R=[pek, msk], W=[pmk])
                        pts.append((pm, kpar))
                    for j in range(4):
                        hd = 2 * (4 * g + j) + half
                        for kb in range(2):
                            pm, kpar = pts[kb]
                            op("tensor", "matmul", dict(out=acc[:, hd * 64:(hd + 1) * 64], lhsT=pm[:, j * 128:(j + 1) * 128], rhs=VB[:, kpar, g * 64:(g + 1) * 64],
                                                        start=(kb == 0), stop=(kb == 1)), R=[G[1], VB], W=[ACCK[hd // 8]])
                        for kb in range(2):
                            pm, kpar = pts[kb]
                            op("tensor", "matmul", dict(out=acc[:, 1024 + hd:1025 + hd], lhsT=pm[:, j * 128:(j + 1) * 128], rhs=onesb[:, 0:1],
                                                        start=(kb == 0), stop=(kb == 1)), R=[G[1], onesb], W=[ACCK[2]])
            den = sm[:, 112:128]
            op("vector", "tensor_tensor", dict(out=den, in0=acc[:, 1024:1040], in1=esink[:], op=ALU.add), R=[ACCK[2], esink], W=[sm])
            op("vector", "reciprocal", dict(out=den, in_=den), R=[sm], W=[sm])
            op("vector", "tensor_tensor", dict(out=cat[:, 1024:2048].rearrange("p (h d) -> p h d", d=64), in0=acc[:, 0:1024].rearrange("p (h d) -> p h d", d=64),
                                               in1=bc(den.unsqueeze(2), [128, 16, 64]), op=ALU.mult), R=[ACCK[0], ACCK[1], sm], W=[cacc])
            transposes_f32(lambda j: szT[:, j, :], 8, lambda j, c: B6[:, 1024 + j * 128:1024 + (j + c) * 128], [B6], [B6])
            op("vector", "tensor_tensor", dict(out=cat[:, 0:1024], in0=cat[:, 0:1024], in1=sz, op=ALU.mult), R=[cacc, B6], W=[cacc])
            st = sm[:, 128:152]; mv = sm[:, 152:156]; rs = sm[:, 156:158]
            for i in range(4):
                op("vector", "bn_stats", dict(out=st[:, i * 6:(i + 1) * 6], in_=cat[:, i * 512:(i + 1) * 512]), R=[cacc], W=[sm])
            for i in range(2):
                op("vector", "bn_aggr", dict(out=mv[:, 2 * i:2 * i + 2], in_=st[:, i * 12:(i + 1) * 12]), R=[sm], W=[sm])
                op("vector", "tensor_tensor", dict(out=rs[:, i:i + 1], in0=mv[:, 2 * i:2 * i + 1], in1=mv[:, 2 * i:2 * i + 1], op=ALU.mult), R=[sm], W=[sm])
                op("vector", "tensor_tensor", dict(out=rs[:, i:i + 1], in0=rs[:, i:i + 1], in1=mv[:, 2 * i + 1:2 * i + 2], op=ALU.add), R=[sm], W=[sm])
            rstd_from(rs, rs, sm)
            catb = ctmp[:].bitcast(BF16)
            for i in range(2):
                op("vector", "tensor_scalar", dict(out=catb[:, i * 1024:(i + 1) * 1024], in0=cat[:, i * 1024:(i + 1) * 1024], scalar1=rs[:, i:i + 1], scalar2=None, op0=ALU.mult),
                   R=[cacc, sm], W=[ctmp])
            catT = XA[:].bitcast(BF16)[:, 0:2048].rearrange("p (k t) -> p k t", t=128)
            for half in range(2):
                b, bk = rb()
                for k in range(8):
                    kc = half * 8 + k
                    op("tensor", "transpose", dict(out=rgb[:, b * 1024 + k * 128:b * 1024 + (k + 1) * 128], in_=catb[:, kc * 128:(kc + 1) * 128], identity=idb[:]),
                       R=[ctmp, idb], W=bk)
                op("scalar", "copy", dict(out=XA[:].bitcast(BF16)[:, half * 1024:(half + 1) * 1024], in_=rgb[:, b * 1024:(b + 1) * 1024]), R=bk, W=[XA])
            for cbk in range(8):
                w = ld_wout(cbk)
                for kc in range(16):
                    op("tensor", "matmul", dict(out=acc[:, cbk * 256:(cbk + 1) * 256], lhsT=catT[:, kc, :], rhs=w[:, kc, :], start=(kc == 0), stop=(kc == 15)),
                       R=[XA, w], W=[ACCK[cbk // 2]])
            v = vv_
            op("vector", "scalar_tensor_tensor", dict(out=v[:], in0=xtl[:], scalar=ALPHA, in1=acc[:], op0=ALU.mult, op1=ALU.add), R=[xtl] + ACCK, W=[v])
            for i in range(4):
                op("vector", "bn_stats", dict(out=st[:, i * 6:(i + 1) * 6], in_=v[:, i * 512:(i + 1) * 512]), R=[v], W=[sm])
            op("vector", "bn_aggr", dict(out=mv[:, 0:2], in_=st), R=[sm], W=[sm])
            rstd_from(mv[:, 1:2], rs[:, 0:1], sm)
            op("vector", "tensor_scalar", dict(out=v[:], in0=v[:], scalar1=mv[:, 0:1], scalar2=rs[:, 0:1], op0=ALU.subtract, op1=ALU.mult), R=[v, sm], W=[v])

        def peer_tile(tau):
            R_, Dm, cacc, ctmp, XA, B5, B6, x1n = Bs
            for j0 in range(0, 16, 4):
                b, bk = rb()
                for k in range(4):
                    kc = j0 + k
                    op("tensor", "transpose", dict(out=rg[:, b * 512 + k * 128:b * 512 + (k + 1) * 128], in_=x1n[:, kc * 128:(kc + 1) * 128], identity=idn[:]),
                       R=[x1n, idn], W=bk)
                for k in range(4):
                    kc = j0 + k
                    op("scalar", "activation", dict(out=h2T[:, kc, :], in_=rg[:, b * 512 + k * 128:b * 512 + (k + 1) * 128], func=AF.Identity,
                                                    scale=A2[:, kc:kc + 1], bias=B2[:, kc:kc + 1]), R=bk + [A2, B2], W=[HTK[kc]])
            QT = R_[:].rearrange("p (a b) -> p a b", b=128)
            QTK = [("QT", i) for i in range(16)]; SCK = [("SC", i) for i in range(16)]; MXK = [("mx8", i) for i in range(16)]
            VTK = [[("Vt", i, j) for j in range(2)] for i in range(16)]; WKK = [("wk", i) for i in range(16)]
            BVK = [[("BV", i, j) for j in range(2)] for i in range(8)]; WK2 = [("wk2", i) for i in range(8)]
            fence([R_], QTK)
            for qb in range(16):
                w = ld_wq(qb)
                q, qk = rq()
                for kc in range(16):
                    op("tensor", "matmul", dict(out=rg[:, q * 128:(q + 1) * 128], lhsT=w[:, kc, :], rhs=h2T[:, kc, :], start=(kc == 0), stop=(kc == 15)),
                       R=[w, HTK[kc]], W=qk)
                op("vector", "tensor_copy", dict(out=QT[:, qb, :], in_=rg[:, q * 128:(q + 1) * 128]), R=qk, W=[QTK[qb]])
            SC = Dm[:].rearrange("p (a b) -> p a b", b=128)
            fence([Dm], SCK)
            for hc in range(16):
                q, qk = rq()
                op("tensor", "matmul", dict(out=rg[:, q * 128:(q + 1) * 128], lhsT=QT[:, hc, :], rhs=KT[:, hc, :], start=True, stop=True), R=[QTK[hc], KT], W=qk)
                op("vector", "tensor_copy", dict(out=SC[:, hc, :], in_=rg[:, q * 128:(q + 1) * 128]), R=qk, W=[SCK[hc]])
            fence(QTK, [R_])
            for hc in range(16):
                op("vector", "max", dict(out=mx8[:, hc, :], in_=SC[:, hc, :]), R=[SCK[hc]], W=[MXK[hc]])
            E = cacc[:].rearrange("p (a b) -> p a b", b=128)
            op("vector", "tensor_tensor", dict(out=E, in0=SC, in1=bc(mx8[:, :, 0:1], [128, 16, 128]), op=ALU.subtract), R=SCK + MXK, W=[cacc, Dm])
            op("scalar", "activation", dict(out=cacc[:], in_=cacc[:], func=AF.Exp), R=[cacc], W=[cacc])
            wk16 = B5[:].rearrange("p (a b) -> p a b", b=128)
            fence([B5], WKK)
            for hc in range(16):
                op("vector", "max", dict(out=Vt[:, hc, 0:8], in_=E[:, hc, :]), R=[cacc], W=[VTK[hc][0]])
            for hc in range(16):
                op("vector", "match_replace", dict(out=wk16[:, hc, :], in_to_replace=Vt[:, hc, 0:8], in_values=E[:, hc, :], imm_value=-1.0), R=[cacc, VTK[hc][0]], W=[WKK[hc]])
            for hc in range(16):
                op("vector", "max", dict(out=Vt[:, hc, 8:16], in_=wk16[:, hc, :]), R=[WKK[hc]], W=[VTK[hc][1]])
            fence(WKK, [B5])
            cand = ctmp[:].rearrange("p (h a b) -> p h a b", h=8, a=16)
            V4 = Vt[:].rearrange("p (h c) k -> p h c k", c=2)
            op("vector", "tensor_tensor", dict(out=cand, in0=bc(V4[:, :, 0, :].unsqueeze(3), [128, 8, 16, 16]), in1=bc(V4[:, :, 1, :].unsqueeze(2), [128, 8, 16, 16]), op=ALU.mult),
               R=[k for kk in VTK for k in kk], W=[ctmp])
            candf = ctmp[:].rearrange("p (h n) -> p h n", h=8)
            wk2 = B6[:].rearrange("p (h n) -> p h n", h=8)
            fence([B6], WK2)
            for h in range(8):
                op("vector", "max", dict(out=BVt[:, h, 0:8], in_=candf[:, h, :]), R=[ctmp], W=[BVK[h][0]])
            for h in range(8):
                op("vector", "match_replace", dict(out=wk2[:, h, :], in_to_replace=BVt[:, h, 0:8], in_values=candf[:, h, :], imm_value=-1.0), R=[ctmp, BVK[h][0]], W=[WK2[h]])
            for h in range(8):
                op("vector", "max", dict(out=BVt[:, h, 8:16], in_=wk2[:, h, :]), R=[WK2[h]], W=[BVK[h][1]])
            fence(WK2, [B6])
            BVall = [k for kk in BVK for k in kk]
            th = sm[:, 160:168]; rz = sm[:, 168:176]
            op("vector", "tensor_scalar", dict(out=th, in0=BVt[:, :, 15], scalar1=0.999999, scalar2=None, op0=ALU.mult), R=BVall, W=[sm])
            op("vector", "tensor_reduce", dict(out=rz, in_=BVt[:], axis=AX.X, op=ALU.add), R=BVall, W=[sm])
            op("vector", "reciprocal", dict(out=rz, in_=rz), R=[sm], W=[sm])
            E4 = cacc[:].rearrange("p (h c n) -> p h c n", h=8, c=2)
            E0v = E4[:, :, 0, :]
            op("vector", "tensor_tensor", dict(out=E0v, in0=E0v, in1=bc(rz.unsqueeze(2), [128, 8, 128]), op=ALU.mult), R=[cacc, sm], W=[cacc])
            op("vector", "tensor_tensor", dict(out=th, in0=th, in1=rz, op=ALU.mult), R=[sm], W=[sm])
            Pb = [Pm[:, i * 512:(i + 1) * 512] for i in range(2)]
            Mmb = Mm[:].bitcast(BF16); M2b = M2[:]
            Mb = [Mmb[:, i * 512:(i + 1) * 512] for i in range(4)] + [M2b[:, i * 512:(i + 1) * 512] for i in range(4)]
            PK = ["Pk0", "Pk1"]; MK = ["Mk%d" % i for i in range(8)]
            fence([Pm, Mm], PK + MK)
            nstep = [0]
            hc_ = 0
            sc_["ut"] = 0; sc_["vv"] = 0

            def ymm(i2p, vblp):
                for ci in range(4):
                    vblk, vk = vblp[ci // 2]
                    for cg in range(4):
                        op("tensor", "matmul", dict(out=acc[:, cg * 512:(cg + 1) * 512], lhsT=wgT[i2p][:, ci, :], rhs=vblk[:, ci % 2, cg * 512:(cg + 1) * 512],
                                                    start=(nstep[0] == 0), stop=(nstep[0] == 127)), R=[wgT[i2p], vk], W=[ACCK[cg]])
                    nstep[0] += 1

            prev = None
            for gb in range(32):
                gbank, gk = rb()
                for h in range(8):
                    k = hc_ % 2; hc_ += 1
                    op("gpsimd", "tensor_tensor", dict(out=Pb[k].rearrange("p (i j) -> p i j", j=128), in0=bc(E4[:, h, 0, gb * 4:(gb + 1) * 4].unsqueeze(2), [128, 4, 128]),
                                                       in1=bc(E4[:, h, 1, :].unsqueeze(1), [128, 4, 128]), op=ALU.mult), R=[cacc], W=[PK[k]])
                    op("vector", "scalar_tensor_tensor", dict(out=Mb[h], in0=Pb[k], scalar=th[:, h:h + 1], in1=Pb[k], op0=ALU.is_ge, op1=ALU.mult), R=[PK[k], sm], W=[MK[h]])
                i2 = gb % 2
                b, bk = rb()
                for sb in range(2):
                    eb = gb * 2 + sb
                    u, uk = ld_ut(eb)
                    for kc in range(16):
                        op("tensor", "matmul", dict(out=rg[:, b * 512 + sb * 256:b * 512 + (sb + 1) * 256], lhsT=h2T[:, kc, :], rhs=u[:, kc, :], start=(kc == 0), stop=(kc == 15)),
                           R=[HTK[kc], uk], W=bk)
                for h in range(8):
                    op("tensor", "matmul", dict(out=rg[:, gbank * 512:(gbank + 1) * 512], lhsT=idb[:], rhs=Mb[h], start=(h == 0), stop=(h == 7)), R=[idb, MK[h]], W=gk)
                op("scalar", "activation", dict(out=gel[i2][:], in_=rg[:, b * 512:(b + 1) * 512], func=AF.Gelu), R=bk, W=[gel[i2]])
                op("vector", "tensor_tensor", dict(out=wg[i2][:], in0=rg[:, gbank * 512:(gbank + 1) * 512], in1=gel[i2][:], op=ALU.mult), R=[gel[i2]] + gk, W=[wg[i2]])
                if prev is not None:
                    ymm(*prev)
                b2, bk2 = rb()
                for ci in range(4):
                    op("tensor", "transpose", dict(out=rgb[:, b2 * 1024 + ci * 128:b2 * 1024 + (ci + 1) * 128], in_=wg[i2][:, ci * 128:(ci + 1) * 128], identity=idb[:]),
                       R=[wg[i2], idb], W=bk2)
                op("scalar", "copy", dict(out=wgT[i2][:].rearrange("p c t -> p (c t)"), in_=rgb[:, b2 * 1024:b2 * 1024 + 512]), R=bk2, W=[wgT[i2]])
                vbl = [ld_vv(gb * 2), ld_vv(gb * 2 + 1)]
                prev = (i2, vbl)
            ymm(*prev)
            fence(PK + MK, [Pm, Mm])
            g1t, b1t, g2t, b2t = R_, Dm, XA, B5
            dma(g1t[:], l1g.partition_broadcast(128), W=[g1t], key="bc0")
            dma(b1t[:], l1b.partition_broadcast(128), W=[b1t], key="bc1")
            dma(g2t[:], l2g.partition_broadcast(128), W=[g2t], key="bc2")
            dma(b2t[:], l2b.partition_broadcast(128), W=[b2t], key="bc3")
            v2 = B6
            op("vector", "tensor_tensor", dict(out=v2[:], in0=x1n[:], in1=g1t[:], op=ALU.mult), R=[x1n, g1t], W=[v2])
            op("vector", "tensor_tensor", dict(out=v2[:], in0=v2[:], in1=b1t[:], op=ALU.add), R=[v2, b1t], W=[v2])
            op("vector", "scalar_tensor_tensor", dict(out=v2[:], in0=v2[:], scalar=ALPHA, in1=acc[:], op0=ALU.mult, op1=ALU.add), R=[v2] + ACCK, W=[v2])
            st = sm[:, 128:152]; mv = sm[:, 152:156]; rs = sm[:, 156:158]
            for i in range(4):
                op("vector", "bn_stats", dict(out=st[:, i * 6:(i + 1) * 6], in_=v2[:, i * 512:(i + 1) * 512]), R=[v2], W=[sm])
            op("vector", "bn_aggr", dict(out=mv[:, 0:2], in_=st), R=[sm], W=[sm])
            rstd_from(mv[:, 1:2], rs[:, 0:1], sm)
            op("vector", "tensor_scalar", dict(out=v2[:], in0=v2[:], scalar1=mv[:, 0:1], scalar2=rs[:, 0:1], op0=ALU.subtract, op1=ALU.mult), R=[v2, sm], W=[v2])
            op("vector", "tensor_tensor", dict(out=v2[:], in0=v2[:], in1=g2t[:], op=ALU.mult), R=[v2, g2t], W=[v2])
            op("vector", "tensor_tensor", dict(out=v2[:], in0=v2[:], in1=b2t[:], op=ALU.add), R=[v2, b2t], W=[v2])
            dma(out[tau * T:(tau + 1) * T, :], v2[:], R=[v2], key="o%d" % (tau % 2), q="scalar")

        for tau in range(-npre, nt):
            mixer_tile(tau)
            if tau >= 0:
                if stop == "mix":
                    dma(out[tau * T:(tau + 1) * T, :], Bs[7][:], R=[Bs[7]], key="o%d" % (tau % 2), q="scalar")
                    continue
                peer_tile(tau)
        P.emit(final_wait=["o0", "o1"] if nt > 1 else ["o0"])
    return nc


def host_prep(inp, nt=16, npre=16, ncores=8, seq=4096):
    f = np.float32
    w_in = inp["w_in"][0]
    cols = []
    cols += [w_in[:, Z0:Z0 + 1024], w_in[:, XS0:XS0 + 1536]]
    cols += [w_in[:, Q0:Q0 + 1024]]
    k0 = w_in[:, K0:K0 + 64]; k1 = w_in[:, K0 + 64:K0 + 128]
    cols += [k0, k1, k1, k0, w_in[:, V0:V0 + 128], w_in[:, DT0:DT0 + 16], np.zeros((D, 112), f)]
    wp = np.ascontiguousarray(np.concatenate(cols, axis=1))
    assert wp.shape == (D, 4096)
    cw = np.ascontiguousarray(inp["conv_w"][0].T.reshape(12, 128, 4).transpose(1, 0, 2))
    cb = np.ascontiguousarray(inp["conv_b"][0].reshape(12, 128).T)
    fm = lambda v: np.ascontiguousarray(v.reshape(16, 128).T)
    gcat = fm(np.concatenate([inp["ssm_norm_g"][0], inp["attn_norm_g"][0]]))
    ii = np.arange(128)
    tri = (ii[:, None] <= ii[None, :]).astype(f)
    mst = (ii[:, None] > ii[None, :]).astype(f)
    shared = dict(idn=np.eye(128, dtype=f), tri=tri, mst=mst, w_ada=inp["w_ada"][0], b_ada=inp["b_ada"][0], wp=wp, convw=cw, convb=cb,
                  dt_bias=inp["dt_bias"][0], a_log=inp["a_log"][0], d_skip=inp["d_skip"][0], sinks=inp["attn_sinks"][0], gcat=gcat,
                  w_out=inp["w_out"][0], ln1_g=inp["ln1_g"][0], ln1_b=inp["ln1_b"][0], l1gT=fm(inp["ln1_g"][0]), l1bT=fm(inp["ln1_b"][0]),
                  w_q=inp["peer_w_q"][0], keys=np.ascontiguousarray(inp["peer_sub_keys"][0].reshape(16, 128, 128)),
                  peer_u=inp["peer_u"][0], peer_v=inp["peer_v"][0], ln2_g=inp["ln2_g"][0], ln2_b=inp["ln2_b"][0])
    x = inp["x"]; c = inp["c"]
    per_seq = seq // (nt * T)
    maps = []
    for core in range(ncores):
        b = core // per_seq; part = core % per_seq
        s0 = part * nt * T
        m = dict(shared)
        m["xm"] = np.ascontiguousarray(x[b, s0:s0 + nt * T])
        if part == 0:
            m["xp"] = np.zeros((max(npre, 1) * T, D), f)
            m["flag"] = np.zeros((128, 1), f)
        else:
            m["xp"] = np.ascontiguousarray(x[b, s0 - npre * T:s0])
            m["flag"] = np.ones((128, 1), f)
        m["cT"] = fm(c[b])
        maps.append(m)
    return maps


def kernel(**inputs):
    inp = {k: np.asarray(v) for k, v in inputs.items()}
    nc = build(16, 16)
    maps = host_prep(inp)
    res = run_bass_kernel_spmd(nc, maps, core_ids=list(range(8)))
    outs = [r["out"] for r in res.results]
    y = np.stack(outs, 0).reshape(4, 4096, D).astype(np.float32)
    return y
```

```python
import contextlib
import numpy as np
import concourse.bass as bass
import concourse.mybir as mybir

F32 = mybir.dt.float32
BF16 = mybir.dt.bfloat16
AF = mybir.ActivationFunctionType
ALU = mybir.AluOpType
AX = mybir.AxisListType

ENGS = ("tensor", "vector", "scalar", "gpsimd", "sync")


class Prog:
    def __init__(self, nc):
        self.nc = nc
        self.ops = []
        self.last_w = {}
        self.readers = {}
        self.eng_last = {e: None for e in ENGS}
        self.dma_keys = {}

    def op(self, eng, fn, reads=(), writes=(), dma=None, multi_w=False):
        i = len(self.ops)
        deps = set()
        reads = [r if isinstance(r, (str, tuple)) else r.name for r in reads]
        writes = [w if isinstance(w, (str, tuple)) else w.name for w in writes]
        for r in reads:
            deps.update(self.last_w.get(r, ()))
        for w in writes:
            deps.update(self.last_w.get(w, ()))
            deps.update(self.readers.get(w, ()))
        for r in reads:
            self.readers.setdefault(r, []).append(i)
        for w in writes:
            if multi_w:
                self.last_w.setdefault(w, []).append(i)
            else:
                self.last_w[w] = [i]
            self.readers[w] = []
        deps.discard(i)
        self.ops.append(dict(eng=eng, fn=fn, deps=deps, dma=dma))
        if dma is not None:
            self.dma_keys.setdefault(dma, []).append(i)
        return i

    def barrier(self):
        allprev = set(range(len(self.ops)))
        need = set()
        for e in ENGS:
            for j in range(len(self.ops) - 1, -1, -1):
                if self.ops[j]["eng"] == e and self.ops[j]["dma"] is None:
                    need.add(j)
                    break
        for k, lst in self.dma_keys.items():
            need.add(lst[-1])
        for e in ENGS:
            i = len(self.ops)
            self.ops.append(dict(eng=e, fn=None, deps=set(need), dma=None))
        self.last_w = {}
        self.readers = {}

    def emit(self, final_wait=()):
        nc = self.nc
        ops = self.ops
        sig = [False] * len(ops)
        for i, o in enumerate(ops):
            for d in o["deps"]:
                od = ops[d]
                if od["dma"] is not None:
                    continue
                if od["eng"] == "tensor" and o["eng"] == "tensor" and o["dma"] is None:
                    continue
                sig[d] = True
        cnt = {e: 0 for e in ENGS}
        sigval = {}
        for i, o in enumerate(ops):
            if o["dma"] is not None:
                continue
            if sig[i]:
                cnt[o["eng"]] += 1
                sigval[i] = cnt[o["eng"]]
        dmaval = {}
        for k, lst in self.dma_keys.items():
            for n, i in enumerate(lst):
                dmaval[i] = 16 * (n + 1)
        with contextlib.ExitStack() as st:
            esem = {e: st.enter_context(nc.semaphore("s_" + e)) for e in ENGS}
            dsem = {k: st.enter_context(nc.semaphore("d_%d" % n)) for n, k in enumerate(self.dma_keys)}
            block = st.enter_context(nc.Block())
            per = {e: [i for i, o in enumerate(ops) if o["eng"] == e] for e in ENGS}

            def run(e, eng):
                known = {}
                for i in per[e]:
                    o = ops[i]
                    waits = {}
                    for d in o["deps"]:
                        od = ops[d]
                        if od["dma"] is not None:
                            s, v = dsem[od["dma"]], dmaval[d]
                        else:
                            if od["fn"] is None:
                                continue
                            if od["eng"] == "tensor" and e == "tensor" and o["dma"] is None:
                                continue
                            s, v = esem[od["eng"]], sigval[d]
                        key = id(s)
                        if waits.get(key, (None, 0))[1] < v:
                            waits[key] = (s, v)
                    for key, (s, v) in waits.items():
                        if known.get(key, 0) < v:
                            eng.wait_ge(s, v)
                            known[key] = v
                    if o["fn"] is None:
                        continue
                    ins = o["fn"](eng)
                    if o["dma"] is not None:
                        ins.then_inc(dsem[o["dma"]], 16)
                    elif sig[i]:
                        ins.then_inc(esem[e], 1)
                if e == "sync":
                    for k in final_wait:
                        lst = self.dma_keys[k]
                        eng.wait_ge(dsem[k], 16 * len(lst))

            @block.tensor
            def _(eng):
                run("tensor", eng)

            @block.vector
            def _(eng):
                run("vector", eng)

            @block.scalar
            def _(eng):
                run("scalar", eng)

            @block.gpsimd
            def _(eng):
                run("gpsimd", eng)

            @block.sync
            def _(eng):
                run("sync", eng)


from concourse.bass_utils import run_bass_kernel_spmd

D = 2048
KC = 16
T = 128
ALPHA = 2.0 ** 0.25
EPS = 1e-5
NEB = 64
Z0, XS0, B0_, C0_, DT0, Q0, K0, V0 = 0, 1024, 2048, 2304, 2560, 2576, 3600, 3728
BZ, BXS, BB, BC, BQ, BKA, BKB, BV, BDT = 0, 8, 16, 18, 20, 28, 29, 30, 31


def build(nt=16, npre=16, stop=None):
    nc = bass.Bass("TRN2", target_bir_lowering=False)
    P = Prog(nc)
    ntok = nt * T
    din = lambda n, s, d=F32: nc.dram_tensor(n, list(s), d, kind="ExternalInput").ap()
    xm = din("xm", [ntok, D]); xp = din("xp", [max(npre, 1) * T, D])
    cT = din("cT", [128, 16]); flag = din("flag", [128, 1])
    idn_d = din("idn", [128, 128]); tri_d = din("tri", [128, 128]); mst_d = din("mst", [128, 128])
    w_ada = din("w_ada", [D, 6 * D]); b_ada = din("b_ada", [6 * D])
    wp = din("wp", [D, 4096]); convw = din("convw", [128, 12, 4]); convb = din("convb", [128, 12])
    dtb = din("dt_bias", [16]); alog = din("a_log", [16]); dsk = din("d_skip", [16]); snk = din("sinks", [16])
    gcat = din("gcat", [128, 16]); w_out = din("w_out", [D, D])
    l1g = din("ln1_g", [D]); l1b = din("ln1_b", [D]); l1gT = din("l1gT", [128, 16]); l1bT = din("l1bT", [128, 16])
    w_q = din("w_q", [D, D]); keys = din("keys", [16, 128, 128])
    pu = din("peer_u", [16384, D]); pv = din("peer_v", [16384, D])
    l2g = din("ln2_g", [D]); l2b = din("ln2_b", [D])
    out = nc.dram_tensor("out", [ntok, D], F32, kind="ExternalOutput").ap()
    dint = lambda n, s: nc.dram_tensor(n, list(s), BF16, kind="Internal").ap()
    WIN = dint("WIN", [32, 128, 16, 128]); WOUT = dint("WOUT", [8, 128, 16, 256]); WQ = dint("WQ", [16, 128, 16, 128])
    UT = dint("UT", [NEB, 128, 16, 256]); VV = dint("VV", [NEB, 128, 2, D])

    ctr = [0]

    def nm(p):
        ctr[0] += 1
        return "%s_%d" % (p, ctr[0])

    def op(eng, name, kw, R=(), W=(), **k2):
        return P.op(eng, lambda e: getattr(e, name)(**kw), reads=R, writes=W, **k2)

    def dma(outap, inap, R=(), W=(), key=None, q="sync", **k2):
        return P.op(q, lambda e: e.dma_start(out=outap, in_=inap), reads=R, writes=W, dma=key, **k2)

    def bc(ap, shape):
        return ap.broadcast_to(list(shape))

    with contextlib.ExitStack() as top:
        TT_ = lambda n, s, d=F32: top.enter_context(nc.sbuf_tensor("sb_" + n, list(s), d))
        acc = top.enter_context(nc.psum_tensor("acc", [128, 2048], F32))
        rg = top.enter_context(nc.psum_tensor("rg", [128, 2048], F32))
        rgb = rg[:].bitcast(BF16)
        rstate = dict(n=0)

        def rq():
            n = rstate["n"]; rstate["n"] = n + 1
            b = n % 4; qq = (n // 4) % 4
            return b * 4 + qq, [("rg", b)]

        def rb():
            n = rstate["n"]; rstate["n"] = n + 1
            b = n % 4
            return b, [("rg", b)]

        ACCK = [("acc", j) for j in range(4)]
        idn = TT_("idn", [128, 128]); tri = TT_("tri", [128, 128]); mst = TT_("mst", [128, 128])
        idb = TT_("idb", [128, 128], BF16); mprevF = TT_("mprevF", [128, 128]); ones = TT_("ones", [128, 128])
        onesb = TT_("onesb", [128, 1], BF16); flg = TT_("flg", [128, 1])
        modT = TT_("modT", [128, 64])
        A2 = TT_("A2", [128, 16]); B2 = TT_("B2", [128, 16])
        KT = TT_("KT", [128, 16, 128])
        cw = TT_("cw", [128, 12, 4]); cb = TT_("cb", [128, 12])
        dtb_bc = TT_("dtb_bc", [128, 16]); A_bc = TT_("A_bc", [128, 16]); dsk_bc = TT_("dsk_bc", [128, 16])
        esink = TT_("esink", [128, 16])
        dma(idn[:], idn_d, W=[idn], key="c0"); dma(tri[:], tri_d, W=[tri], key="c1"); dma(mst[:], mst_d, W=[mst], key="c2")
        dma(flg[:], flag, W=[flg], key="c3"); dma(cw[:], convw, W=[cw], key="c4"); dma(cb[:], convb, W=[cb], key="c5")
        dma(dtb_bc[:], dtb.partition_broadcast(128), W=[dtb_bc], key="c6")
        dma(A_bc[:], alog.partition_broadcast(128), W=[A_bc], key="c7")
        dma(dsk_bc[:], dsk.partition_broadcast(128), W=[dsk_bc], key="c8")
        dma(esink[:], snk.partition_broadcast(128), W=[esink], key="c9")
        op("vector", "tensor_copy", dict(out=idb[:], in_=idn[:]), R=[idn], W=[idb])
        op("vector", "memset", dict(ap=ones[:], constant=1.0), W=[ones])
        op("vector", "memset", dict(ap=onesb[:], constant=1.0), W=[onesb])
        op("vector", "tensor_scalar", dict(out=mprevF[:], in0=mst[:], scalar1=flg[:, 0:1], scalar2=None, op0=ALU.mult), R=[mst, flg], W=[mprevF])
        op("scalar", "activation", dict(out=A_bc[:], in_=A_bc[:], func=AF.Exp), R=[A_bc], W=[A_bc])
        op("vector", "tensor_scalar", dict(out=A_bc[:], in0=A_bc[:], scalar1=-1.0, scalar2=None, op0=ALU.mult), R=[A_bc], W=[A_bc])
        op("scalar", "activation", dict(out=esink[:], in_=esink[:], func=AF.Exp), R=[esink], W=[esink])

        with contextlib.ExitStack() as pro:
            PT = lambda n, s, d=F32: pro.enter_context(nc.sbuf_tensor("sp_" + n, list(s), d))
            cTt = PT("cTt", [128, 16]); SCb = PT("SCb", [128, 16, 128])
            g1bc = PT("g1bc", [128, D]); g2bc = PT("g2bc", [128, D]); gct = PT("gct", [128, 16])
            wab = [PT("wab%d" % i, [128, 4, 512]) for i in range(3)]
            bab = [PT("bab%d" % i, [128, 512]) for i in range(2)]
            mrow = [PT("mrow%d" % i, [128, 512]) for i in range(2)]
            st32 = [PT("st32_%d" % i, [128, 4096]) for i in range(2)]
            st16 = [PT("st16_%d" % i, [128, 4096], BF16) for i in range(2)]
            ut16 = [PT("ut16_%d" % i, [128, 4096], BF16) for i in range(2)]
            l1gt = PT("l1gt", [128, 16]); l1bt = PT("l1bt", [128, 16])
            dma(cTt[:], cT, W=[cTt], key="p0"); dma(gct[:], gcat, W=[gct], key="p1")
            dma(l1gt[:], l1gT, W=[l1gt], key="p2"); dma(l1bt[:], l1bT, W=[l1bt], key="p3")
            op("scalar", "activation", dict(out=cTt[:], in_=cTt[:], func=AF.Silu), R=[cTt], W=[cTt])
            op("vector", "tensor_copy", dict(out=SCb[:], in_=bc(cTt[:].unsqueeze(2), [128, 16, 128])), R=[cTt], W=[SCb])

            def finish_early():
                dma(out[0:128, 0:64], modT[:], R=[modT], key="o0", q="scalar")
                dma(out[0:128, 64:80], A2[:], R=[A2], key="o0", q="scalar")
                dma(out[0:128, 80:96], B2[:], R=[B2], key="o0", q="scalar")
                dma(out[0:128, 128:256], KT[:, 3, :], R=[KT], key="o0", q="scalar")
                P.barrier()
                P.emit(final_wait=["o0"])
                return nc
            if stop == "pA":
                return finish_early()
            for hc in range(16):
                kb_ = st32[hc % 2]
                dma(kb_[:, 0:128], keys[hc], W=[kb_], key="pk%d" % (hc % 2))
                q, qk = rq()
                op("tensor", "transpose", dict(out=rg[:, q * 128:(q + 1) * 128], in_=kb_[:, 0:128], identity=idn[:]), R=[kb_, idn], W=qk)
                op("vector", "tensor_copy", dict(out=KT[:, hc, :], in_=rg[:, q * 128:(q + 1) * 128]), R=qk, W=[KT])
            if stop == "p0":
                return finish_early()
            for g in range(24):
                b, bk = rb()
                bb = bab[g % 2]
                dma(bb[:], b_ada[g * 512:(g + 1) * 512].partition_broadcast(128), W=[bb], key="pb%d" % (g % 2))
                for kq in range(4):
                    wb = wab[(g * 4 + kq) % 3]
                    dma(wb[:], w_ada[kq * 512:(kq + 1) * 512, g * 512:(g + 1) * 512].rearrange("(k p) c -> p k c", p=128),
                        W=[wb], key="pw%d" % ((g * 4 + kq) % 3))
                    for k in range(4):
                        kc = kq * 4 + k
                        op("tensor", "matmul", dict(out=rg[:, b * 512:(b + 1) * 512], lhsT=SCb[:, kc, :], rhs=wb[:, k, :],
                                                    start=(kc == 0), stop=(kc == 15)), R=[SCb, wb], W=bk)
                gi = g // 4
                if gi == 2:
                    dst, dk = g1bc[:, (g % 4) * 512:(g % 4 + 1) * 512], g1bc
                elif gi == 5:
                    dst, dk = g2bc[:, (g % 4) * 512:(g % 4 + 1) * 512], g2bc
                else:
                    mr = mrow[g % 2]
                    dst, dk = mr[:], mr
                op("vector", "tensor_tensor", dict(out=dst, in0=rg[:, b * 512:(b + 1) * 512], in1=bb[:], op=ALU.add), R=bk + [bb], W=[dk])
                if gi in (0, 1, 3, 4):
                    col0 = {0: 0, 1: 16, 3: 32, 4: 48}[gi] + (g % 4) * 4
                    b2, bk2 = rb()
                    for j in range(4):
                        op("tensor", "transpose", dict(out=rg[:, b2 * 512 + j * 128: b2 * 512 + (j + 1) * 128], in_=mr[:, j * 128:(j + 1) * 128], identity=idn[:]),
                           R=[mr, idn], W=bk2)
                    src = rg[:, b2 * 512:(b2 + 1) * 512].rearrange("p (j t) -> p j t", t=128)[:, :, 0]
                    if gi in (1, 4):
                        op("vector", "tensor_scalar", dict(out=modT[:, col0:col0 + 4], in0=src, scalar1=1.0, scalar2=None, op0=ALU.add), R=bk2, W=[modT])
                    else:
                        op("vector", "tensor_copy", dict(out=modT[:, col0:col0 + 4], in_=src), R=bk2, W=[modT])
            op("vector", "tensor_tensor", dict(out=A2[:], in0=l1gt[:], in1=modT[:, 48:64], op=ALU.mult), R=[l1gt, modT], W=[A2])
            op("vector", "tensor_tensor", dict(out=B2[:], in0=l1bt[:], in1=modT[:, 48:64], op=ALU.mult), R=[l1bt, modT], W=[B2])
            op("vector", "tensor_tensor", dict(out=B2[:], in0=B2[:], in1=modT[:, 32:48], op=ALU.add), R=[B2, modT], W=[B2])
            if stop == "p1":
                return finish_early()
            pc = [0]

            def stage(src_ap, shape3, mul_bc=None, mul_pp=None, dst=None, dkey=None):
                i = pc[0] % 2; pc[0] += 1
                s32, s16 = st32[i], st16[i]
                n = shape3[1] * shape3[2]
                v32 = s32[:, 0:n].rearrange("p (a b) -> p a b", b=shape3[2])
                v16 = s16[:, 0:n].rearrange("p (a b) -> p a b", b=shape3[2])
                dma(v32, src_ap, W=[s32], key="pl%d" % i)
                if mul_bc is not None:
                    op("vector", "tensor_tensor", dict(out=v32, in0=v32, in1=mul_bc, op=ALU.mult), R=[s32, g1bc, g2bc], W=[s32])
                if mul_pp is not None:
                    op("gpsimd", "tensor_tensor", dict(out=v16, in0=v32, in1=mul_pp, op=ALU.mult), R=[s32, gct], W=[s16])
                else:
                    op("scalar", "copy", dict(out=v16, in_=v32), R=[s32], W=[s16])
                if dst is not None:
                    dma(dst, v16, R=[s16], W=[dkey], key="ps%d" % i, q="scalar", multi_w=True)
                return s16, v16

            for b in range(32):
                stage(wp[:, b * 128:(b + 1) * 128].rearrange("(k p) c -> p k c", p=128), [128, 16, 128], dst=WIN[b], dkey="WIN")
            if stop == "p2":
                return finish_early()
            for b in range(16):
                stage(w_q[:, b * 128:(b + 1) * 128].rearrange("(k p) c -> p k c", p=128), [128, 16, 128], dst=WQ[b], dkey="WQ")
            for b in range(8):
                stage(w_out[:, b * 256:(b + 1) * 256].rearrange("(k p) c -> p k c", p=128), [128, 16, 256],
                      mul_bc=bc(g1bc[:, b * 256:(b + 1) * 256].unsqueeze(1), [128, 16, 256]),
                      mul_pp=bc(gct[:].unsqueeze(2), [128, 16, 256]), dst=WOUT[b], dkey="WOUT")
            if stop == "p3":
                return finish_early()
            for b in range(NEB):
                stage(pv[b * 256:(b + 1) * 256, :].rearrange("(c p) d -> p c d", p=128), [128, 2, D],
                      mul_bc=bc(g2bc[:].unsqueeze(1), [128, 2, D]), dst=VV[b], dkey="VV")
            if stop == "p4":
                return finish_early()
            for b in range(NEB):
                s16, v16 = stage(pu[b * 256:(b + 1) * 256, :].rearrange("(c p) d -> p c d", p=128), [128, 2, D])
                u16 = ut16[b % 2]
                uv = u16[:].rearrange("p (k e) -> p k e", e=256)
                for kg in range(4):
                    bnk, bk = rb()
                    for k in range(4):
                        kc = kg * 4 + k
                        for ci in range(2):
                            o0 = bnk * 1024 + k * 256 + ci * 128
                            op("tensor", "transpose", dict(out=rgb[:, o0:o0 + 128], in_=v16[:, ci, kc * 128:(kc + 1) * 128], identity=idb[:]),
                               R=[s16, idb], W=bk)
                    op("vector", "tensor_copy", dict(out=u16[:, kg * 1024:(kg + 1) * 1024], in_=rgb[:, bnk * 1024:(bnk + 1) * 1024]), R=bk, W=[u16])
                dma(UT[b], uv, R=[u16], W=["UT"], key="pu%d" % (b % 2), q="scalar", multi_w=True)
            P.barrier()
            if stop == "pro":
                dma(out[0:128, 0:64], modT[:], R=[modT], key="o0", q="scalar")
                dma(out[0:128, 64:80], A2[:], R=[A2], key="o0", q="scalar")
                dma(out[0:128, 80:96], B2[:], R=[B2], key="o0", q="scalar")
                P.emit(final_wait=["o0"])
                return nc
        MT = TT_
        xt = [MT("xt%d" % i, [128, D]) for i in range(1)]
        win_r = [MT("winr%d" % i, [128, 16, 128], BF16) for i in range(3)]
        wout_r = [MT("woutr%d" % i, [128, 16, 256], BF16) for i in range(2)]
        wq_r = [MT("wqr%d" % i, [128, 16, 128], BF16) for i in range(3)]
        ut_r = [MT("utr%d" % i, [128, 16, 256], BF16) for i in range(2)]
        vv_r = [MT("vvr%d" % i, [128, 2, D], BF16) for i in range(2)]
        hT = MT("hT", [128, 16, 128], BF16); h2T = hT
        XC = MT("XC", [128, 12, 131]); S = MT("S", [128, 1024])
        KTb = MT("KTb", [128, 2, 2, 128], BF16)
        VB = MT("VB", [128, 2, 128], BF16)
        Bs = [MT("BIG%d" % i, [128, D]) for i in range(8)]
        sm = MT("sm", [128, 256])
        Vt = MT("Vt", [128, 16, 16]); BVt = MT("BVt", [128, 8, 16]); wk = MT("wk", [128, 256])
        mx8 = MT("mx8", [128, 16, 8])
        G = [MT("G0", [128, 1024])] * 2
        Pm = MT("Pm", [128, 1024]); Mm = MT("Mm", [128, 1024]); M2 = MT("M2", [128, 2048], BF16)
        gel = [MT("gel%d" % i, [128, 512]) for i in range(2)]
        wg = [MT("wg%d" % i, [128, 512], BF16) for i in range(2)]
        wgT = [MT("wgT%d" % i, [128, 4, 128], BF16) for i in range(2)]
        G0b = G[0][:].bitcast(BF16); G1b = G0b[:, 1024:2048]
        qT = G0b[:, 0:1024].rearrange("p (a b) -> p a b", b=128); qTk = G[0]
        pTm = [(G1b[:, 0:512], G[1]), (G1b[:, 512:1024], G[1])]
        pTe = [(Pm[:, 512:1024], Pm), (Mm[:, 512:1024], Mm)]
        fz = MT("fz", [128, 2])
        op("vector", "memset", dict(ap=fz[:], constant=0.0))
        op("vector", "memset", dict(ap=sm[:], constant=0.0), W=[sm])
        HTK = [("hT", kc) for kc in range(16)]
        XCK = [("XC", j) for j in range(12)]

        def fence(R, W):
            op("vector", "tensor_copy", dict(out=fz[:, 1:2], in_=fz[:, 0:1]), R=R, W=W)
        op("vector", "memset", dict(ap=XC[:], constant=0.0), W=XCK)
        op("vector", "memset", dict(ap=S[:], constant=0.0), W=[S])
        op("vector", "memset", dict(ap=KTb[:], constant=0.0), W=[KTb])
        op("vector", "memset", dict(ap=VB[:], constant=0.0), W=[VB])

        sc_ = dict(win=0, wout=0, wq=0, ut=0, vv=0, x=0)

        def ld_win(b):
            i = sc_["win"] % 3; sc_["win"] += 1
            dma(win_r[i][:], WIN[b], R=["WIN"], W=[win_r[i]], key="win%d" % i)
            return win_r[i]

        def ld_wout(b):
            i = sc_["wout"] % 2; sc_["wout"] += 1
            dma(wout_r[i][:], WOUT[b], R=["WOUT"], W=[wout_r[i]], key="wout%d" % i)
            return wout_r[i]

        def ld_wq(b):
            i = sc_["wq"] % 3; sc_["wq"] += 1
            dma(wq_r[i][:], WQ[b], R=["WQ"], W=[wq_r[i]], key="wq%d" % i)
            return wq_r[i]

        def _bf(t):
            return t[:].bitcast(BF16)
        ut_slots = [(ut_r[0][:], ut_r[0]), (ut_r[1][:], ut_r[1])] + \
                   [(_bf(Bs[i]).rearrange("p (k e) -> p k e", e=256), Bs[i]) for i in (1, 3, 4)]
        vv_slots = [(vv_r[0][:], vv_r[0]), (vv_r[1][:], vv_r[1])] + \
                   [(_bf(Bs[i]).rearrange("p (c d) -> p c d", d=D), Bs[i]) for i in (5, 6, 0)]

        def ld_ut(b):
            i = sc_["ut"] % 5; sc_["ut"] += 1
            view, kt = ut_slots[i]
            dma(view, UT[b], R=["UT"], W=[kt], key="ut%d" % i)
            return view, kt

        def ld_vv(b):
            i = sc_["vv"] % 5; sc_["vv"] += 1
            view, kt = vv_slots[i]
            dma(view, VV[b], R=["VV"], W=[kt], key="vv%d" % i)
            return view, kt

        def rstd_from(var_ap, out_ap, keyt):
            op("vector", "tensor_scalar", dict(out=out_ap, in0=var_ap, scalar1=EPS, scalar2=None, op0=ALU.add), R=[keyt], W=[keyt])
            op("scalar", "activation", dict(out=out_ap, in_=out_ap, func=AF.Ln), R=[keyt], W=[keyt])
            op("scalar", "activation", dict(out=out_ap, in_=out_ap, func=AF.Exp, scale=-0.5), R=[keyt], W=[keyt])

        def transposes_f32(src_fn, n, dst_fn, dkeys, rkeys, evac="vector"):
            j = 0
            while j < n:
                cnt = min(4, n - j)
                b, bk = rb()
                for k in range(cnt):
                    op("tensor", "transpose", dict(out=rg[:, b * 512 + k * 128: b * 512 + (k + 1) * 128], in_=src_fn(j + k), identity=idn[:]),
                       R=rkeys + [idn], W=bk)
                if evac == "vector":
                    op("vector", "tensor_copy", dict(out=dst_fn(j, cnt), in_=rg[:, b * 512: b * 512 + cnt * 128]), R=bk, W=dkeys)
                else:
                    op("scalar", "copy", dict(out=dst_fn(j, cnt), in_=rg[:, b * 512: b * 512 + cnt * 128]), R=bk, W=dkeys)
                j += cnt

        def mixer_tile(tau):
            full = tau >= 0
            par = tau % 2
            xsrc = xm[tau * T:(tau + 1) * T, :] if full else xp[(npre + tau) * T:(npre + tau + 1) * T, :]
            xi = 0
            xtl = xt[xi]
            dma(xtl[:], xsrc, W=[xtl], key="x%d" % xi)
            if tau == 0:
                op("vector", "tensor_scalar", dict(out=S[:], in0=S[:], scalar1=flg[:, 0:1], scalar2=None, op0=ALU.mult), R=[S, flg], W=[S])
                op("vector", "tensor_scalar", dict(out=XC[:], in0=XC[:], scalar1=flg[:, 0:1], scalar2=None, op0=ALU.mult), R=XCK + [flg], W=XCK)
            for j0 in range(0, 16, 4):
                b, bk = rb()
                for k in range(4):
                    kc = j0 + k
                    op("tensor", "transpose", dict(out=rg[:, b * 512 + k * 128:b * 512 + (k + 1) * 128], in_=xtl[:, kc * 128:(kc + 1) * 128], identity=idn[:]),
                       R=[xtl, idn], W=bk)
                for k in range(4):
                    kc = j0 + k
                    op("scalar", "activation", dict(out=hT[:, kc, :], in_=rg[:, b * 512 + k * 128:b * 512 + (k + 1) * 128], func=AF.Identity,
                                                    scale=modT[:, 16 + kc:17 + kc], bias=modT[:, kc:kc + 1]), R=bk + [modT], W=[HTK[kc]])
            R_, Dm, cacc, ctmp, XA, B5, B6, vv_ = Bs
            XS = B5[:, 0:1024]; XSd = B5[:, 1024:2048]
            szT = B6[:, 0:1024].rearrange("p (a b) -> p a b", b=128); sz = B6[:, 1024:2048]
            XAv = XA[:, 0:1536].rearrange("p (a b) -> p a b", b=128)

            def proj(blk):
                w = ld_win(blk)
                q, qk = rq()
                for kc in range(16):
                    op("tensor", "matmul", dict(out=rg[:, q * 128:(q + 1) * 128], lhsT=w[:, kc, :], rhs=hT[:, kc, :], start=(kc == 0), stop=(kc == 15)),
                       R=[w, HTK[kc]], W=qk)
                return rg[:, q * 128:(q + 1) * 128], qk

            nconv = 12 if (full or tau == -1) else 10
            for j in range(nconv):
                ps, qk = proj(BXS + j)
                op("vector", "tensor_copy", dict(out=XC[:, j, 3:131], in_=ps), R=qk, W=[XCK[j]])
            ps, qk = proj(BDT)
            dtT = Pm[:, 0:128]
            op("vector", "tensor_copy", dict(out=dtT, in_=ps), R=qk, W=[Pm])
            q, qk = rq()
            op("tensor", "transpose", dict(out=rg[:, q * 128:(q + 1) * 128], in_=dtT, identity=idn[:]), R=[Pm, idn], W=qk)
            dt = sm[:, 0:16]; dtA = sm[:, 16:32]; acum = sm[:, 32:48]; alast = sm[:, 48:64]; dte = sm[:, 64:80]; ea = sm[:, 80:96]; cd = sm[:, 96:112]
            op("vector", "tensor_tensor", dict(out=dt, in0=rg[:, q * 128:q * 128 + 16], in1=dtb_bc[:], op=ALU.add), R=qk + [dtb_bc], W=[sm])
            op("scalar", "activation", dict(out=dt, in_=dt, func=AF.Exp), R=[sm], W=[sm])
            op("vector", "tensor_scalar", dict(out=dt, in0=dt, scalar1=1.0, scalar2=None, op0=ALU.add), R=[sm], W=[sm])
            op("scalar", "activation", dict(out=dt, in_=dt, func=AF.Ln), R=[sm], W=[sm])
            op("vector", "tensor_tensor", dict(out=dtA, in0=dt, in1=A_bc[:], op=ALU.mult), R=[sm, A_bc], W=[sm])
            q, qk = rq()
            op("tensor", "matmul", dict(out=rg[:, q * 128:q * 128 + 16], lhsT=tri[:], rhs=dtA, start=True, stop=True), R=[tri, sm], W=qk)
            op("tensor", "matmul", dict(out=rg[:, q * 128 + 16:q * 128 + 32], lhsT=ones[:], rhs=dtA, start=True, stop=True), R=[ones, sm], W=qk)
            op("vector", "tensor_copy", dict(out=sm[:, 32:64], in_=rg[:, q * 128:q * 128 + 32]), R=qk, W=[sm])
            op("vector", "tensor_tensor", dict(out=dte, in0=alast, in1=acum, op=ALU.subtract), R=[sm], W=[sm])
            op("scalar", "activation", dict(out=dte, in_=dte, func=AF.Exp), R=[sm], W=[sm])
            op("vector", "tensor_tensor", dict(out=dte, in0=dte, in1=dt, op=ALU.mult), R=[sm], W=[sm])
            op("scalar", "activation", dict(out=ea, in_=acum, func=AF.Exp), R=[sm], W=[sm])
            op("scalar", "activation", dict(out=cd, in_=alast, func=AF.Exp), R=[sm], W=[sm])
            if full:
                for j in range(8):
                    ps, qk = proj(BZ + j)
                    op("scalar", "activation", dict(out=szT[:, j, :], in_=ps, func=AF.Silu), R=qk, W=[B6])
                for j in range(8):
                    ps, qk = proj(BQ + j)
                    op("scalar", "activation", dict(out=qT[:, j, :], in_=ps, func=AF.Identity, scale=0.125), R=qk, W=[qTk])
            if full or tau == -1:
                for ab, blk in ((0, BKA), (1, BKB)):
                    ps, qk = proj(blk)
                    op("scalar", "copy", dict(out=KTb[:, ab, par, :], in_=ps), R=qk, W=[KTb])
                ps, qk = proj(BV)
                vT = Pm[:, 128:256]
                op("scalar", "copy", dict(out=vT, in_=ps), R=qk, W=[Pm])
                q, qk = rq()
                op("tensor", "transpose", dict(out=rg[:, q * 128:(q + 1) * 128], in_=vT, identity=idn[:]), R=[Pm, idn], W=qk)
                op("scalar", "copy", dict(out=VB[:, par, :], in_=rg[:, q * 128:(q + 1) * 128]), R=qk, W=[VB])
            ca = cacc[:, 0:1536].rearrange("p (a b) -> p a b", b=128)[:, 0:nconv, :]
            ct = ctmp[:, 0:1536].rearrange("p (a b) -> p a b", b=128)[:, 0:nconv, :]
            for k in range(4):
                dst, dk = (ca, cacc) if k == 0 else (ct, ctmp)
                op("vector", "tensor_tensor", dict(out=dst, in0=XC[:, 0:nconv, k:k + 128], in1=bc(cw[:, 0:nconv, k:k + 1], [128, nconv, 128]), op=ALU.mult),
                   R=XCK[0:nconv] + [cw], W=[dk])
                if k > 0:
                    op("vector", "tensor_tensor", dict(out=ca, in0=ca, in1=ct, op=ALU.add), R=[cacc, ctmp], W=[cacc])
            op("vector", "tensor_tensor", dict(out=ca, in0=ca, in1=bc(cb[:, 0:nconv].unsqueeze(2), [128, nconv, 128]), op=ALU.add), R=[cacc, cb], W=[cacc])
            op("scalar", "activation", dict(out=XAv[:, 0:nconv, :], in_=ca, func=AF.Silu), R=[cacc], W=[XA])
            op("vector", "tensor_copy", dict(out=XC[:, :, 0:3], in_=XC[:, :, 128:131]), R=XCK, W=XCK)
            transposes_f32(lambda j: XAv[:, j, :], 8, lambda j, c: B5[:, j * 128:(j + c) * 128], [B5], [XA])
            Btok = sm[:, 0:256]
            Btok = Mm[:, 0:256]
            transposes_f32(lambda j: XAv[:, 8 + j, :], 2, lambda j, c: Mm[:, j * 128:(j + c) * 128], [Mm], [XA])
            if full:
                Rv = R_[:].rearrange("p (h l) -> p h l", l=128)
                op("vector", "tensor_tensor", dict(out=Rv, in0=bc(tri[:].unsqueeze(1), [128, 16, 128]), in1=bc(dtA.unsqueeze(2), [128, 16, 128]), op=ALU.mult),
                   R=[tri, sm], W=[R_])
                for hq in range(4):
                    b, bk = rb()
                    op("tensor", "matmul", dict(out=rg[:, b * 512:(b + 1) * 512], lhsT=mst[:], rhs=R_[:, hq * 512:(hq + 1) * 512], start=True, stop=True),
                       R=[mst, R_], W=bk)
                    op("scalar", "activation", dict(out=Dm[:, hq * 512:(hq + 1) * 512], in_=rg[:, b * 512:(b + 1) * 512], func=AF.Exp), R=bk, W=[Dm])
                q0, qk0 = rq()
                q1, qk1 = rq()
                cbm = Mm[:, 256:512].rearrange("p (g l) -> p g l", l=128)
                for g, (q, qk) in enumerate(((q0, qk0), (q1, qk1))):
                    op("tensor", "matmul", dict(out=rg[:, q * 128:(q + 1) * 128], lhsT=XAv[:, 8 + g, :], rhs=XAv[:, 10 + g, :], start=True, stop=True),
                       R=[XA], W=qk)
                    op("vector", "tensor_tensor", dict(out=cbm[:, g, :], in0=rg[:, q * 128:(q + 1) * 128], in1=tri[:], op=ALU.mult), R=qk + [tri], W=[Mm])
                Dv = Dm[:].rearrange("p (g r l) -> p g r l", g=2, r=8)
                op("vector", "tensor_tensor", dict(out=Dv, in0=Dv, in1=bc(cbm.unsqueeze(2), [128, 2, 8, 128]), op=ALU.mult), R=[Dm, Mm], W=[Dm])
                Dv3 = Dm[:].rearrange("p (h l) -> p h l", l=128)
                op("vector", "tensor_tensor", dict(out=Dv3, in0=Dv3, in1=bc(dt.unsqueeze(2), [128, 16, 128]), op=ALU.mult), R=[Dm, sm], W=[Dm])
                for h in range(16):
                    g = h // 8
                    op("tensor", "matmul", dict(out=acc[:, h * 64:(h + 1) * 64], lhsT=Dv3[:, h, :], rhs=XS[:, h * 64:(h + 1) * 64], start=True, stop=True),
                       R=[Dm, B5], W=[ACCK[h // 8]])
                for h in range(16):
                    g = h // 8
                    op("tensor", "matmul", dict(out=acc[:, 1024 + h * 64:1024 + (h + 1) * 64], lhsT=XAv[:, 10 + g, :], rhs=S[:, h * 64:(h + 1) * 64], start=True, stop=True),
                       R=[XA, S], W=[ACCK[2 + h // 8]])
                cat = cacc
                catv = cat[:, 0:1024].rearrange("p (h d) -> p h d", d=64)
                tmpv = ctmp[:, 0:1024].rearrange("p (h d) -> p h d", d=64)
                op("vector", "tensor_tensor", dict(out=tmpv, in0=acc[:, 1024:2048].rearrange("p (h d) -> p h d", d=64), in1=bc(ea.unsqueeze(2), [128, 16, 64]), op=ALU.mult),
                   R=[ACCK[2], ACCK[3], sm], W=[ctmp])
                op("vector", "tensor_tensor", dict(out=cat[:, 0:1024], in0=acc[:, 0:1024], in1=ctmp[:, 0:1024], op=ALU.add), R=[ACCK[0], ACCK[1], ctmp], W=[cacc])
                op("vector", "tensor_tensor", dict(out=tmpv, in0=XS.rearrange("p (h d) -> p h d", d=64), in1=bc(dsk_bc[:].unsqueeze(2), [128, 16, 64]), op=ALU.mult),
                   R=[B5, dsk_bc], W=[ctmp])
                op("vector", "tensor_tensor", dict(out=cat[:, 0:1024], in0=cat[:, 0:1024], in1=ctmp[:, 0:1024], op=ALU.add), R=[cacc, ctmp], W=[cacc])
            op("vector", "tensor_tensor", dict(out=XSd.rearrange("p (h d) -> p h d", d=64), in0=XS.rearrange("p (h d) -> p h d", d=64),
                                               in1=bc(dte.unsqueeze(2), [128, 16, 64]), op=ALU.mult), R=[B5, sm], W=[B5])
            b0, bk0 = rb()
            b1, bk1 = rb()
            for h in range(16):
                g = h // 8
                bnk, bk = (b0, bk0) if h < 8 else (b1, bk1)
                o0 = bnk * 512 + (h % 8) * 64
                op("tensor", "matmul", dict(out=rg[:, o0:o0 + 64], lhsT=Btok[:, g * 128:(g + 1) * 128], rhs=XSd[:, h * 64:(h + 1) * 64], start=True, stop=True),
                   R=[Mm, B5], W=bk)
            Sv = S[:].rearrange("p (h d) -> p h d", d=64)
            op("vector", "tensor_tensor", dict(out=Sv, in0=Sv, in1=bc(cd.unsqueeze(2), [128, 16, 64]), op=ALU.mult), R=[S, sm], W=[S])
            op("vector", "tensor_tensor", dict(out=S[:, 0:512], in0=S[:, 0:512], in1=rg[:, b0 * 512:(b0 + 1) * 512], op=ALU.add), R=[S] + bk0, W=[S])
            op("vector", "tensor_tensor", dict(out=S[:, 512:1024], in0=S[:, 512:1024], in1=rg[:, b1 * 512:(b1 + 1) * 512], op=ALU.add), R=[S] + bk1, W=[S])
            if not full:
                return
            for g in range(2):
                for half in range(2):
                    ab = {(0, 0): 0, (0, 1): 1, (1, 0): 1, (1, 1): 0}[(g, half)]
                    pts = []
                    for kb in range(2):
                        kpar = par if kb == 1 else 1 - par
                        b, bk = rb()
                        op("tensor", "matmul", dict(out=rg[:, b * 512:(b + 1) * 512], lhsT=KTb[64 * half:64 * half + 64, ab, kpar, :],
                                                    rhs=qT[64 * half:64 * half + 64, 4 * g:4 * g + 4, :], start=True, stop=True), R=[KTb, qTk], W=bk)
                        (pe, pek), (pm, pmk) = pTe[kb], pTm[kb]
                        op("scalar", "activation", dict(out=pe, in_=rg[:, b * 512:(b + 1) * 512], func=AF.Exp), R=bk, W=[pek])
                        msk = tri if kb == 1 else (mprevF if tau == 0 else mst)
                        op("vector", "tensor_tensor", dict(out=pm.rearrange("p (j t) -> p j t", t=128), in0=pe.rearrange("p (j t) -> p j t", t=128),
                                                           in1=bc(msk[:].unsqueeze(1), [128, 4, 128]), op=ALU.mult), R=[pek, msk], W=[pmk])
                        pts.append((pm, kpar))
                    for j in range(4):
                        hd = 2 * (4 * g + j) + half
                        for kb in range(2):
                            pm, kpar = pts[kb]
                            op("tensor", "matmul", dict(out=acc[:, hd * 64:(hd + 1) * 64], lhsT=pm[:, j * 128:(j + 1) * 128], rhs=VB[:, kpar, g * 64:(g + 1) * 64],
                                                        start=(kb == 0), stop=(kb == 1)), R=[G[1], VB], W=[ACCK[hd // 8]])
                        for kb in range(2):
                            pm, kpar = pts[kb]
                            op("tensor", "matmul", dict(out=acc[:, 1024 + hd:1025 + hd], lhsT=pm[:, j * 128:(j + 1) * 128], rhs=onesb[:, 0:1],
                                                        start=(kb == 0), stop=(kb == 1)), R=[G[1], onesb], W=[ACCK[2]])
            den = sm[:, 112:128]
            op("vector", "tensor_tensor", dict(out=den, in0=acc[:, 1024:1040], in1=esink[:], op=ALU.add), R=[ACCK[2], esink], W=[sm])
            op("vector", "reciprocal", dict(out=den, in_=den), R=[sm], W=[sm])
            op("vector", "tensor_tensor", dict(out=cat[:, 1024:2048].rearrange("p (h d) -> p h d", d=64), in0=acc[:, 0:1024].rearrange("p (h d) -> p h d", d=64),
                                               in1=bc(den.unsqueeze(2), [128, 16, 64]), op=ALU.mult), R=[ACCK[0], ACCK[1], sm], W=[cacc])
            transposes_f32(lambda j: szT[:, j, :], 8, lambda j, c: B6[:, 1024 + j * 128:1024 + (j + c) * 128], [B6], [B6])
            op("vector", "tensor_tensor", dict(out=cat[:, 0:1024], in0=cat[:, 0:1024], in1=sz, op=ALU.mult), R=[cacc, B6], W=[cacc])
            st = sm[:, 128:152]; mv = sm[:, 152:156]; rs = sm[:, 156:158]
            for i in range(4):
                op("vector", "bn_stats", dict(out=st[:, i * 6:(i + 1) * 6], in_=cat[:, i * 512:(i + 1) * 512]), R=[cacc], W=[sm])
            for i in range(2):
                op("vector", "bn_aggr", dict(out=mv[:, 2 * i:2 * i + 2], in_=st[:, i * 12:(i + 1) * 12]), R=[sm], W=[sm])
                op("vector", "tensor_tensor", dict(out=rs[:, i:i + 1], in0=mv[:, 2 * i:2 * i + 1], in1=mv[:, 2 * i:2 * i + 1], op=ALU.mult), R=[sm], W=[sm])
                op("vector", "tensor_tensor", dict(out=rs[:, i:i + 1], in0=rs[:, i:i + 1], in1=mv[:, 2 * i + 1:2 * i + 2], op=ALU.add), R=[sm], W=[sm])
            rstd_from(rs, rs, sm)
            catb = ctmp[:].bitcast(BF16)
            for i in range(2):
                op("vector", "tensor_scalar", dict(out=catb[:, i * 1024:(i + 1) * 1024], in0=cat[:, i * 1024:(i + 1) * 1024], scalar1=rs[:, i:i + 1], scalar2=None, op0=ALU.mult),
                   R=[cacc, sm], W=[ctmp])
            catT = XA[:].bitcast(BF16)[:, 0:2048].rearrange("p (k t) -> p k t", t=128)
            for half in range(2):
                b, bk = rb()
                for k in range(8):
                    kc = half * 8 + k
                    op("tensor", "transpose", dict(out=rgb[:, b * 1024 + k * 128:b * 1024 + (k + 1) * 128], in_=catb[:, kc * 128:(kc + 1) * 128], identity=idb[:]),
                       R=[ctmp, idb], W=bk)
                op("scalar", "copy", dict(out=XA[:].bitcast(BF16)[:, half * 1024:(half + 1) * 1024], in_=rgb[:, b * 1024:(b + 1) * 1024]), R=bk, W=[XA])
            for cbk in range(8):
                w = ld_wout(cbk)
                for kc in range(16):
                    op("tensor", "matmul", dict(out=acc[:, cbk * 256:(cbk + 1) * 256], lhsT=catT[:, kc, :], rhs=w[:, kc, :], start=(kc == 0), stop=(kc == 15)),
                       R=[XA, w], W=[ACCK[cbk // 2]])
            v = vv_
            op("vector", "scalar_tensor_tensor", dict(out=v[:], in0=xtl[:], scalar=ALPHA, in1=acc[:], op0=ALU.mult, op1=ALU.add), R=[xtl] + ACCK, W=[v])
            for i in range(4):
                op("vector", "bn_stats", dict(out=st[:, i * 6:(i + 1) * 6], in_=v[:, i * 512:(i + 1) * 512]), R=[v], W=[sm])
            op("vector", "bn_aggr", dict(out=mv[:, 0:2], in_=st), R=[sm], W=[sm])
            rstd_from(mv[:, 1:2], rs[:, 0:1], sm)
            op("vector", "tensor_scalar", dict(out=v[:], in0=v[:], scalar1=mv[:, 0:1], scalar2=rs[:, 0:1], op0=ALU.subtract, op1=ALU.mult), R=[v, sm], W=[v])

        def peer_tile(tau):
            R_, Dm, cacc, ctmp, XA, B5, B6, x1n = Bs
            for j0 in range(0, 16, 4):
                b, bk = rb()
                for k in range(4):
                    kc = j0 + k
                    op("tensor", "transpose", dict(out=rg[:, b * 512 + k * 128:b * 512 + (k + 1) * 128], in_=x1n[:, kc * 128:(kc + 1) * 128], identity=idn[:]),
                       R=[x1n, idn], W=bk)
                for k in range(4):
                    kc = j0 + k
                    op("scalar", "activation", dict(out=h2T[:, kc, :], in_=rg[:, b * 512 + k * 128:b * 512 + (k + 1) * 128], func=AF.Identity,
                                                    scale=A2[:, kc:kc + 1], bias=B2[:, kc:kc + 1]), R=bk + [A2, B2], W=[HTK[kc]])
            QT = R_[:].rearrange("p (a b) -> p a b", b=128)
            QTK = [("QT", i) for i in range(16)]; SCK = [("SC", i) for i in range(16)]; MXK = [("mx8", i) for i in range(16)]
            VTK = [[("Vt", i, j) for j in range(2)] for i in range(16)]; WKK = [("wk", i) for i in range(16)]
            BVK = [[("BV", i, j) for j in range(2)] for i in range(8)]; WK2 = [("wk2", i) for i in range(8)]
            fence([R_], QTK)
            for qb in range(16):
                w = ld_wq(qb)
                q, qk = rq()
                for kc in range(16):
                    op("tensor", "matmul", dict(out=rg[:, q * 128:(q + 1) * 128], lhsT=w[:, kc, :], rhs=h2T[:, kc, :], start=(kc == 0), stop=(kc == 15)),
                       R=[w, HTK[kc]], W=qk)
                op("vector", "tensor_copy", dict(out=QT[:, qb, :], in_=rg[:, q * 128:(q + 1) * 128]), R=qk, W=[QTK[qb]])
            SC = Dm[:].rearrange("p (a b) -> p a b", b=128)
            fence([Dm], SCK)
            for hc in range(16):
                q, qk = rq()
                op("tensor", "matmul", dict(out=rg[:, q * 128:(q + 1) * 128], lhsT=QT[:, hc, :], rhs=KT[:, hc, :], start=True, stop=True), R=[QTK[hc], KT], W=qk)
                op("vector", "tensor_copy", dict(out=SC[:, hc, :], in_=rg[:, q * 128:(q + 1) * 128]), R=qk, W=[SCK[hc]])
            fence(QTK, [R_])
            for hc in range(16):
                op("vector", "max", dict(out=mx8[:, hc, :], in_=SC[:, hc, :]), R=[SCK[hc]], W=[MXK[hc]])
            E = cacc[:].rearrange("p (a b) -> p a b", b=128)
            op("vector", "tensor_tensor", dict(out=E, in0=SC, in1=bc(mx8[:, :, 0:1], [128, 16, 128]), op=ALU.subtract), R=SCK + MXK, W=[cacc, Dm])
            op("scalar", "activation", dict(out=cacc[:], in_=cacc[:], func=AF.Exp), R=[cacc], W=[cacc])
            wk16 = B5[:].rearrange("p (a b) -> p a b", b=128)
            fence([B5], WKK)
            for hc in range(16):
                op("vector", "max", dict(out=Vt[:, hc, 0:8], in_=E[:, hc, :]), R=[cacc], W=[VTK[hc][0]])
            for hc in range(16):
                op("vector", "match_replace", dict(out=wk16[:, hc, :], in_to_replace=Vt[:, hc, 0:8], in_values=E[:, hc, :], imm_value=-1.0), R=[cacc, VTK[hc][0]], W=[WKK[hc]])
            for hc in range(16):
                op("vector", "max", dict(out=Vt[:, hc, 8:16], in_=wk16[:, hc, :]), R=[WKK[hc]], W=[VTK[hc][1]])
            fence(WKK, [B5])
            cand = ctmp[:].rearrange("p (h a b) -> p h a b", h=8, a=16)
            V4 = Vt[:].rearrange("p (h c) k -> p h c k", c=2)
            op("vector", "tensor_tensor", dict(out=cand, in0=bc(V4[:, :, 0, :].unsqueeze(3), [128, 8, 16, 16]), in1=bc(V4[:, :, 1, :].unsqueeze(2), [128, 8, 16, 16]), op=ALU.mult),
               R=[k for kk in VTK for k in kk], W=[ctmp])
            candf = ctmp[:].rearrange("p (h n) -> p h n", h=8)
            wk2 = B6[:].rearrange("p (h n) -> p h n", h=8)
            fence([B6], WK2)
            for h in range(8):
                op("vector", "max", dict(out=BVt[:, h, 0:8], in_=candf[:, h, :]), R=[ctmp], W=[BVK[h][0]])
            for h in range(8):
                op("vector", "match_replace", dict(out=wk2[:, h, :], in_to_replace=BVt[:, h, 0:8], in_values=candf[:, h, :], imm_value=-1.0), R=[ctmp, BVK[h][0]], W=[WK2[h]])
            for h in range(8):
                op("vector", "max", dict(out=BVt[:, h, 8:16], in_=wk2[:, h, :]), R=[WK2[h]], W=[BVK[h][1]])
            fence(WK2, [B6])
            BVall = [k for kk in BVK for k in kk]
            th = sm[:, 160:168]; rz = sm[:, 168:176]
            op("vector", "tensor_scalar", dict(out=th, in0=BVt[:, :, 15], scalar1=0.999999, scalar2=None, op0=ALU.mult), R=BVall, W=[sm])
            op("vector", "tensor_reduce", dict(out=rz, in_=BVt[:], axis=AX.X, op=ALU.add), R=BVall, W=[sm])
            op("vector", "reciprocal", dict(out=rz, in_=rz), R=[sm], W=[sm])
            E4 = cacc[:].rearrange("p (h c n) -> p h c n", h=8, c=2)
            E0v = E4[:, :, 0, :]
            op("vector", "tensor_tensor", dict(out=E0v, in0=E0v, in1=bc(rz.unsqueeze(2), [128, 8, 128]), op=ALU.mult), R=[cacc, sm], W=[cacc])
            op("vector", "tensor_tensor", dict(out=th, in0=th, in1=rz, op=ALU.mult), R=[sm], W=[sm])
            Pb = [Pm[:, i * 512:(i + 1) * 512] for i in range(2)]
            Mmb = Mm[:].bitcast(BF16); M2b = M2[:]
            Mb = [Mmb[:, i * 512:(i + 1) * 512] for i in range(4)] + [M2b[:, i * 512:(i + 1) * 512] for i in range(4)]
            PK = ["Pk0", "Pk1"]; MK = ["Mk%d" % i for i in range(8)]
            fence([Pm, Mm], PK + MK)
            nstep = [0]
            hc_ = 0
            sc_["ut"] = 0; sc_["vv"] = 0

            def ymm(i2p, vblp):
                for ci in range(4):
                    vblk, vk = vblp[ci // 2]
                    for cg in range(4):
                        op("tensor", "matmul", dict(out=acc[:, cg * 512:(cg + 1) * 512], lhsT=wgT[i2p][:, ci, :], rhs=vblk[:, ci % 2, cg * 512:(cg + 1) * 512],
                                                    start=(nstep[0] == 0), stop=(nstep[0] == 127)), R=[wgT[i2p], vk], W=[ACCK[cg]])
                    nstep[0] += 1

            prev = None
            for gb in range(32):
                gbank, gk = rb()
                for h in range(8):
                    k = hc_ % 2; hc_ += 1
                    op("gpsimd", "tensor_tensor", dict(out=Pb[k].rearrange("p (i j) -> p i j", j=128), in0=bc(E4[:, h, 0, gb * 4:(gb + 1) * 4].unsqueeze(2), [128, 4, 128]),
                                                       in1=bc(E4[:, h, 1, :].unsqueeze(1), [128, 4, 128]), op=ALU.mult), R=[cacc], W=[PK[k]])
                    op("vector", "scalar_tensor_tensor", dict(out=Mb[h], in0=Pb[k], scalar=th[:, h:h + 1], in1=Pb[k], op0=ALU.is_ge, op1=ALU.mult), R=[PK[k], sm], W=[MK[h]])
                i2 = gb % 2
                b, bk = rb()
                for sb in range(2):
                    eb = gb * 2 + sb
                    u, uk = ld_ut(eb)
                    for kc in range(16):
                        op("tensor", "matmul", dict(out=rg[:, b * 512 + sb * 256:b * 512 + (sb + 1) * 256], lhsT=h2T[:, kc, :], rhs=u[:, kc, :], start=(kc == 0), stop=(kc == 15)),
                           R=[HTK[kc], uk], W=bk)
                for h in range(8):
                    op("tensor", "matmul", dict(out=rg[:, gbank * 512:(gbank + 1) * 512], lhsT=idb[:], rhs=Mb[h], start=(h == 0), stop=(h == 7)), R=[idb, MK[h]], W=gk)
                op("scalar", "activation", dict(out=gel[i2][:], in_=rg[:, b * 512:(b + 1) * 512], func=AF.Gelu), R=bk, W=[gel[i2]])
                op("vector", "tensor_tensor", dict(out=wg[i2][:], in0=rg[:, gbank * 512:(gbank + 1) * 512], in1=gel[i2][:], op=ALU.mult), R=[gel[i2]] + gk, W=[wg[i2]])
                if prev is not None:
                    ymm(*prev)
                b2, bk2 = rb()
                for ci in range(4):
                    op("tensor", "transpose", dict(out=rgb[:, b2 * 1024 + ci * 128:b2 * 1024 + (ci + 1) * 128], in_=wg[i2][:, ci * 128:(ci + 1) * 128], identity=idb[:]),
                       R=[wg[i2], idb], W=bk2)
                op("scalar", "copy", dict(out=wgT[i2][:].rearrange("p c t -> p (c t)"), in_=rgb[:, b2 * 1024:b2 * 1024 + 512]), R=bk2, W=[wgT[i2]])
                vbl = [ld_vv(gb * 2), ld_vv(gb * 2 + 1)]
                prev = (i2, vbl)
            ymm(*prev)
            fence(PK + MK, [Pm, Mm])
            g1t, b1t, g2t, b2t = R_, Dm, XA, B5
            dma(g1t[:], l1g.partition_broadcast(128), W=[g1t], key="bc0")
            dma(b1t[:], l1b.partition_broadcast(128), W=[b1t], key="bc1")
            dma(g2t[:], l2g.partition_broadcast(128), W=[g2t], key="bc2")
            dma(b2t[:], l2b.partition_broadcast(128), W=[b2t], key="bc3")
            v2 = B6
            op("vector", "tensor_tensor", dict(out=v2[:], in0=x1n[:], in1=g1t[:], op=ALU.mult), R=[x1n, g1t], W=[v2])
            op("vector", "tensor_tensor", dict(out=v2[:], in0=v2[:], in1=b1t[:], op=ALU.add), R=[v2, b1t], W=[v2])
            op("vector", "scalar_tensor_tensor", dict(out=v2[:], in0=v2[:], scalar=ALPHA, in1=acc[:], op0=ALU.mult, op1=ALU.add), R=[v2] + ACCK, W=[v2])
            st = sm[:, 128:152]; mv = sm[:, 152:156]; rs = sm[:, 156:158]
            for i in range(4):
                op("vector", "bn_stats", dict(out=st[:, i * 6:(i + 1) * 6], in_=v2[:, i * 512:(i + 1) * 512]), R=[v2], W=[sm])
            op("vector", "bn_aggr", dict(out=mv[:, 0:2], in_=st), R=[sm], W=[sm])
            rstd_from(mv[:, 1:2], rs[:, 0:1], sm)
            op("vector", "tensor_scalar", dict(out=v2[:], in0=v2[:], scalar1=mv[:, 0:1], scalar2=rs[:, 0:1], op0=ALU.subtract, op1=ALU.mult), R=[v2, sm], W=[v2])
            op("vector", "tensor_tensor", dict(out=v2[:], in0=v2[:], in1=g2t[:], op=ALU.mult), R=[v2, g2t], W=[v2])
            op("vector", "tensor_tensor", dict(out=v2[:], in0=v2[:], in1=b2t[:], op=ALU.add), R=[v2, b2t], W=[v2])
            dma(out[tau * T:(tau + 1) * T, :], v2[:], R=[v2], key="o%d" % (tau % 2), q="scalar")

        for tau in range(-npre, nt):
            mixer_tile(tau)
            if tau >= 0:
                if stop == "mix":
                    dma(out[tau * T:(tau + 1) * T, :], Bs[7][:], R=[Bs[7]], key="o%d" % (tau % 2), q="scalar")
                    continue
                peer_tile(tau)
        P.emit(final_wait=["o0", "o1"] if nt > 1 else ["o0"])
    return nc


def host_prep(inp, nt=16, npre=16, ncores=8, seq=4096):
    f = np.float32
    w_in = inp["w_in"][0]
    cols = []
    cols += [w_in[:, Z0:Z0 + 1024], w_in[:, XS0:XS0 + 1536]]
    cols += [w_in[:, Q0:Q0 + 1024]]
    k0 = w_in[:, K0:K0 + 64]; k1 = w_in[:, K0 + 64:K0 + 128]
    cols += [k0, k1, k1, k0, w_in[:, V0:V0 + 128], w_in[:, DT0:DT0 + 16], np.zeros((D, 112), f)]
    wp = np.ascontiguousarray(np.concatenate(cols, axis=1))
    assert wp.shape == (D, 4096)
    cw = np.ascontiguousarray(inp["conv_w"][0].T.reshape(12, 128, 4).transpose(1, 0, 2))
    cb = np.ascontiguousarray(inp["conv_b"][0].reshape(12, 128).T)
    fm = lambda v: np.ascontiguousarray(v.reshape(16, 128).T)
    gcat = fm(np.concatenate([inp["ssm_norm_g"][0], inp["attn_norm_g"][0]]))
    ii = np.arange(128)
    tri = (ii[:, None] <= ii[None, :]).astype(f)
    mst = (ii[:, None] > ii[None, :]).astype(f)
    shared = dict(idn=np.eye(128, dtype=f), tri=tri, mst=mst, w_ada=inp["w_ada"][0], b_ada=inp["b_ada"][0], wp=wp, convw=cw, convb=cb,
                  dt_bias=inp["dt_bias"][0], a_log=inp["a_log"][0], d_skip=inp["d_skip"][0], sinks=inp["attn_sinks"][0], gcat=gcat,
                  w_out=inp["w_out"][0], ln1_g=inp["ln1_g"][0], ln1_b=inp["ln1_b"][0], l1gT=fm(inp["ln1_g"][0]), l1bT=fm(inp["ln1_b"][0]),
                  w_q=inp["peer_w_q"][0], keys=np.ascontiguousarray(inp["peer_sub_keys"][0].reshape(16, 128, 128)),
                  peer_u=inp["peer_u"][0], peer_v=inp["peer_v"][0], ln2_g=inp["ln2_g"][0], ln2_b=inp["ln2_b"][0])
    x = inp["x"]; c = inp["c"]
    per_seq = seq // (nt * T)
    maps = []
    for core in range(ncores):
        b = core // per_seq; part = core % per_seq
        s0 = part * nt * T
        m = dict(shared)
        m["xm"] = np.ascontiguousarray(x[b, s0:s0 + nt * T])
        if part == 0:
            m["xp"] = np.zeros((max(npre, 1) * T, D), f)
            m["flag"] = np.zeros((128, 1), f)
        else:
            m["xp"] = np.ascontiguousarray(x[b, s0 - npre * T:s0])
            m["flag"] = np.ones((128, 1), f)
        m["cT"] = fm(c[b])
        maps.append(m)
    return maps


def kernel(**inputs):
    inp = {k: np.asarray(v) for k, v in inputs.items()}
    nc = build(16, 16)
    maps = host_prep(inp)
    res = run_bass_kernel_spmd(nc, maps, core_ids=list(range(8)))
    outs = [r["out"] for r in res.results]
    y = np.stack(outs, 0).reshape(4, 4096, D).astype(np.float32)
    return y
```
